# Optimizing a Trainium2 kernel written in Bass

```python
import math
import jax, jax.numpy as jnp
from jax import lax
import numpy as np

D_MODEL = 4096
BATCH = 4
SEQ = 4096
DEPTH = 1

HEAD_DIM = 128
N_MOBA_HEADS = D_MODEL // (2 * HEAD_DIM)
N_SB_HEADS = D_MODEL // (2 * HEAD_DIM)
MOBA_WIDTH = N_MOBA_HEADS * HEAD_DIM
SB_WIDTH = N_SB_HEADS * HEAD_DIM
MIX_WIDTH = MOBA_WIDTH + SB_WIDTH
MOBA_BLOCK = 256
MOBA_TOPK = 3
MOBA_Q_CHUNK = 16
SB_Q_BLOCK = 128
N_MEM = 256
N_XATTN_HEADS = 4
XATTN_WIDTH = N_XATTN_HEADS * HEAD_DIM
PEER_HEADS = 8
PEER_N_KEYS = 128
PEER_N_EXPERTS = PEER_N_KEYS * PEER_N_KEYS
PEER_KEY_DIM = 256
PEER_HALF = PEER_KEY_DIM // 2
PEER_TOPK = 16
RMS_EPS = 1e-6
NEG_INF = -1e30

kernel_name = 'hybrid_moba_stickbreaking_peer_layer'


def rmsnorm(x, g):
    xf = x.astype(jnp.float32)
    y = xf * lax.rsqrt(jnp.mean(xf * xf, axis=-1, keepdims=True) + RMS_EPS)
    return (y * g.astype(jnp.float32)).astype(x.dtype)


def alibi_slopes(n_heads):
    return jnp.asarray(2.0 ** (-8.0 * np.arange(1, n_heads + 1) / n_heads), dtype=jnp.float32)


def split_heads(t, n_heads):
    b, s, _ = t.shape
    return t.reshape(b, s, n_heads, HEAD_DIM).transpose(0, 2, 1, 3)


def merge_heads(t):
    b, h, s, d = t.shape
    return t.transpose(0, 2, 1, 3).reshape(b, s, h * d)


def moba_attention(q, k, v, slopes):
    B, H, T, Dh = q.shape
    nb = -(-T // MOBA_BLOCK)
    pad = nb * MOBA_BLOCK - T
    k_blocks = jnp.pad(k, ((0, 0), (0, 0), (0, pad), (0, 0))).reshape(B, H, nb, MOBA_BLOCK, Dh)
    v_blocks = jnp.pad(v, ((0, 0), (0, 0), (0, pad), (0, 0))).reshape(B, H, nb, MOBA_BLOCK, Dh)
    k_mean = jnp.mean(k_blocks.astype(jnp.float32), axis=3)
    scale = Dh ** -0.5
    k_eff = min(MOBA_TOPK, nb)
    b_idx = jnp.arange(B)[:, None, None, None]
    h_idx = jnp.arange(H)[None, :, None, None]
    blk_pos = jnp.arange(MOBA_BLOCK)
    n_chunks = T // MOBA_Q_CHUNK

    def chunk(c):
        t0 = c * MOBA_Q_CHUNK
        q_c = lax.dynamic_slice_in_dim(q, t0, MOBA_Q_CHUNK, axis=2)
        t_pos = t0 + jnp.arange(MOBA_Q_CHUNK)
        own = t0 // MOBA_BLOCK
        gate = jnp.einsum('bhqd,bhnd->bhqn', q_c.astype(jnp.float32), k_mean)
        gate = jnp.where(jnp.arange(nb) < own, gate, NEG_INF)
        _, sel = lax.top_k(gate, k_eff)
        sel_valid = jnp.arange(k_eff) < own
        k_sel = k_blocks[b_idx, h_idx, sel]
        v_sel = v_blocks[b_idx, h_idx, sel]
        s_sel = jnp.einsum('bhqd,bhqkjd->bhqkj', q_c, k_sel).astype(jnp.float32) * scale
        dist_sel = (t_pos[:, None, None] - (sel[..., None] * MOBA_BLOCK + blk_pos)).astype(jnp.float32)
        s_sel = s_sel - slopes[:, None, None, None] * dist_sel
        s_sel = jnp.where(sel_valid[:, None], s_sel, NEG_INF)
        s_sel = s_sel.reshape(B, H, MOBA_Q_CHUNK, k_eff * MOBA_BLOCK)
        k_own = lax.dynamic_index_in_dim(k_blocks, own, axis=2, keepdims=False)
        v_own = lax.dynamic_index_in_dim(v_blocks, own, axis=2, keepdims=False)
        s_own = jnp.einsum('bhqd,bhjd->bhqj', q_c, k_own).astype(jnp.float32) * scale
        dist_own = t_pos[:, None] - (own * MOBA_BLOCK + blk_pos)[None, :]
        s_own = s_own - slopes[:, None, None] * dist_own.astype(jnp.float32)
        s_own = jnp.where(dist_own >= 0, s_own, NEG_INF)
        p = jax.nn.softmax(jnp.concatenate([s_sel, s_own], axis=-1), axis=-1)
        p_sel = p[..., : k_eff * MOBA_BLOCK].reshape(B, H, MOBA_Q_CHUNK, k_eff, MOBA_BLOCK)
        p_own = p[..., k_eff * MOBA_BLOCK:]
        return (jnp.einsum('bhqkj,bhqkjd->bhqd', p_sel.astype(v.dtype), v_sel)
                + jnp.einsum('bhqj,bhjd->bhqd', p_own.astype(v.dtype), v_own))

    o = lax.map(chunk, jnp.arange(n_chunks))
    return jnp.moveaxis(o, 0, 2).reshape(B, H, T, Dh)


def stick_breaking_attention(q, k, v):
    B, H, T, Dh = q.shape
    scale = Dh ** -0.5
    key_pos = jnp.arange(T)
    n_blocks = T // SB_Q_BLOCK

    def block(c):
        t0 = c * SB_Q_BLOCK
        q_c = lax.dynamic_slice_in_dim(q, t0, SB_Q_BLOCK, axis=2)
        t_pos = t0 + jnp.arange(SB_Q_BLOCK)
        z = jnp.einsum('bhqd,bhsd->bhqs', q_c, k).astype(jnp.float32) * scale
        strict = key_pos[None, :] < t_pos[:, None]
        log_beta = jax.nn.log_sigmoid(z)
        log_one_minus = jnp.where(strict, jax.nn.log_sigmoid(-z), 0.0)
        log_remain = lax.cumsum(log_one_minus, axis=3, reverse=True) - log_one_minus
        a = jnp.where(strict, jnp.exp(log_beta + log_remain), 0.0)
        return jnp.einsum('bhqs,bhsd->bhqd', a.astype(v.dtype), v)

    o = lax.map(block, jnp.arange(n_blocks))
    return jnp.moveaxis(o, 0, 2).reshape(B, H, T, Dh)


def cross_attention(h, mem_n, w_xq, w_xkv, q_norm_g, k_norm_g, w_xo):
    B, T, _ = h.shape
    M = mem_n.shape[1]
    q = rmsnorm((h @ w_xq).reshape(B, T, N_XATTN_HEADS, HEAD_DIM), q_norm_g)
    kv = mem_n @ w_xkv
    k = rmsnorm(kv[..., :XATTN_WIDTH].reshape(B, M, N_XATTN_HEADS, HEAD_DIM), k_norm_g)
    v = kv[..., XATTN_WIDTH:].reshape(B, M, N_XATTN_HEADS, HEAD_DIM)
    s = jnp.einsum('bthd,bmhd->bhtm', q, k).astype(jnp.float32) * (HEAD_DIM ** -0.5)
    p = jax.nn.softmax(s, axis=-1)
    o = jnp.einsum('bhtm,bmhd->bthd', p.astype(v.dtype), v).reshape(B, T, XATTN_WIDTH)
    return o @ w_xo


def peer_ffn(h, w_q, sub_keys, u, v):
    B, T, D = h.shape
    n_tok = B * T
    hf = h.reshape(n_tok, D)
    q = (hf @ w_q).reshape(n_tok, PEER_HEADS, 2, PEER_HALF)
    s = jnp.einsum('nhpc,hpkc->nhpk', q, sub_keys).astype(jnp.float32)
    val, idx = lax.top_k(s, PEER_TOPK)
    cand = val[:, :, 0, :, None] + val[:, :, 1, None, :]
    cand_id = idx[:, :, 0, :, None] * PEER_N_KEYS + idx[:, :, 1, None, :]
    top_val, top_pos = lax.top_k(cand.reshape(n_tok, PEER_HEADS, PEER_TOPK * PEER_TOPK), PEER_TOPK)
    expert = jnp.take_along_axis(cand_id.reshape(n_tok, PEER_HEADS, PEER_TOPK * PEER_TOPK), top_pos, axis=-1)
    g = jax.nn.softmax(top_val, axis=-1)
    expert = expert.reshape(n_tok, PEER_HEADS * PEER_TOPK)
    act = jax.nn.gelu(hf @ u.T, approximate=False)
    a_sel = jnp.take_along_axis(act, expert, axis=-1)
    w = (g.reshape(n_tok, -1) * a_sel.astype(jnp.float32)).astype(act.dtype)
    gate = jnp.zeros_like(act).at[jnp.arange(n_tok)[:, None], expert].add(w)
    return (gate @ v).reshape(B, T, D)


def setup_inputs(seed: int = 0) -> dict:
    key = jax.random.key(seed)
    ks = jax.random.split(key, 21)
    L, D = DEPTH, D_MODEL

    def normal(k, shape, scale):
        return jax.random.normal(k, shape, jnp.float32) * scale

    def gain(k, shape):
        return 1.0 + 0.02 * jax.random.normal(k, shape, jnp.float32)

    return {
        'x': normal(ks[0], (BATCH, SEQ, D), 1.0),
        'mem': normal(ks[1], (BATCH, N_MEM, D), 1.0),
        'norm_mix_g': gain(ks[2], (L, D)),
        'w_in': normal(ks[3], (L, D, 3 * MIX_WIDTH), D ** -0.5),
        'moba_q_norm_g': gain(ks[4], (L, HEAD_DIM)),
        'moba_k_norm_g': gain(ks[5], (L, HEAD_DIM)),
        'moba_out_norm_g': gain(ks[6], (L, MOBA_WIDTH)),
        'sb_out_norm_g': gain(ks[7], (L, SB_WIDTH)),
        'w_out': normal(ks[8], (L, MIX_WIDTH, D), MIX_WIDTH ** -0.5),
        'norm_xattn_g': gain(ks[9], (L, D)),
        'norm_mem_g': gain(ks[10], (L, D)),
        'w_xq': normal(ks[11], (L, D, XATTN_WIDTH), D ** -0.5),
        'w_xkv': normal(ks[12], (L, D, 2 * XATTN_WIDTH), D ** -0.5),
        'xattn_q_norm_g': gain(ks[13], (L, HEAD_DIM)),
        'xattn_k_norm_g': gain(ks[14], (L, HEAD_DIM)),
        'w_xo': normal(ks[15], (L, XATTN_WIDTH, D), XATTN_WIDTH ** -0.5),
        'norm_ffn_g': gain(ks[16], (L, D)),
        'w_peer_q': normal(ks[17], (L, D, PEER_HEADS * PEER_KEY_DIM), D ** -0.5),
        'peer_sub_keys': normal(ks[18], (L, PEER_HEADS, 2, PEER_N_KEYS, PEER_HALF), PEER_HALF ** -0.5),
        'peer_u': normal(ks[19], (L, PEER_N_EXPERTS, D), D ** -0.5),
        'peer_v': normal(ks[20], (L, PEER_N_EXPERTS, D), PEER_HEADS ** -0.5),
    }


def reference(x, mem, norm_mix_g, w_in, moba_q_norm_g, moba_k_norm_g, moba_out_norm_g,
              sb_out_norm_g, w_out, norm_xattn_g, norm_mem_g, w_xq, w_xkv, xattn_q_norm_g,
              xattn_k_norm_g, w_xo, norm_ffn_g, w_peer_q, peer_sub_keys, peer_u, peer_v):
    slopes = alibi_slopes(N_MOBA_HEADS)
    splits = [MOBA_WIDTH, 2 * MOBA_WIDTH, 3 * MOBA_WIDTH,
              3 * MOBA_WIDTH + SB_WIDTH, 3 * MOBA_WIDTH + 2 * SB_WIDTH]
    for l in range(DEPTH):
        h = rmsnorm(x, norm_mix_g[l])
        proj = h @ w_in[l]
        mq, mk, mv, sq, sk, sv = jnp.split(proj, splits, axis=-1)
        mq = rmsnorm(split_heads(mq, N_MOBA_HEADS), moba_q_norm_g[l])
        mk = rmsnorm(split_heads(mk, N_MOBA_HEADS), moba_k_norm_g[l])
        o_moba = moba_attention(mq, mk, split_heads(mv, N_MOBA_HEADS), slopes)
        o_sb = stick_breaking_attention(split_heads(sq, N_SB_HEADS), split_heads(sk, N_SB_HEADS),
                                        split_heads(sv, N_SB_HEADS))
        o_moba = rmsnorm(merge_heads(o_moba), moba_out_norm_g[l])
        o_sb = rmsnorm(merge_heads(o_sb), sb_out_norm_g[l])
        x = x + jnp.concatenate([o_moba, o_sb], axis=-1) @ w_out[l]
        h = rmsnorm(x, norm_xattn_g[l])
        mem_n = rmsnorm(mem, norm_mem_g[l])
        x = x + cross_attention(h, mem_n, w_xq[l], w_xkv[l], xattn_q_norm_g[l],
                                xattn_k_norm_g[l], w_xo[l])
        h = rmsnorm(x, norm_ffn_g[l])
        x = x + peer_ffn(h, w_peer_q[l], peer_sub_keys[l], peer_u[l], peer_v[l])
    return x
```

```python
import math
from contextlib import ExitStack
import numpy as np
import ml_dtypes
import concourse.bass as bass
import concourse.mybir as mybir
from concourse.bass_utils import run_bass_kernel_spmd

F32 = mybir.dt.float32
BF16 = mybir.dt.bfloat16
AF = mybir.ActivationFunctionType
ALU = mybir.AluOpType
AX = mybir.AxisListType

RMS_EPS = 1e-6
BIG = 30000.0


class Cfg:
    def __init__(s, D=4096, T=4096, B=4, NMEM=256):
        s.D, s.T, s.B, s.NMEM = D, T, B, NMEM
        s.DC = D // 128
        s.HM = D // 256
        s.HS = D // 256
        s.MIX = D
        s.NB = T // 256
        s.NOWN = s.NB // 2
        s.TQ = T // 2
        s.XW = 512
        s.PH = 8
        s.NK = 128
        s.NE = 128 * 128
        s.PW = 8 * 256
        s.ncores = 2 * B


def own_blocks(cfg, p):
    out = []
    for i in range(cfg.NOWN):
        a, b = 2 * i, 2 * i + 1
        if i % 2 == 0:
            out.append(a if p == 0 else b)
        else:
            out.append(b if p == 0 else a)
    return out


class Buf:
    __slots__ = ("name", "last_w", "readers", "excl")

    def __init__(s, name, excl=False):
        s.name = name
        s.last_w = None
        s.readers = {}
        s.excl = excl


class Phase:
    ENGS = ("pe", "act", "dve", "pool", "sp")
    NDMA = {"sp": 8, "pool": 4, "act": 2}

    def __init__(s, nc, name):
        s.nc = nc
        s.name = name
        s.ops = {e: [] for e in s.ENGS}
        s.cnt = {e: 0 for e in s.ENGS}
        s.seen = {e: {} for e in s.ENGS}
        s.dcnt = {}
        s.drot = {e: 0 for e in s.ENGS}

    def op(s, eng, fn, reads=(), writes=(), dma=False):
        deps = {}

        def add(ev):
            if ev is None:
                return
            k, v = ev
            if deps.get(k, 0) < v:
                deps[k] = v

        for b in reads:
            add(b.last_w)
            if b.excl:
                for k, v in b.readers.items():
                    add((k, v))
        for b in writes:
            add(b.last_w)
            for k, v in b.readers.items():
                add((k, v))
        waits = []
        for k, v in deps.items():
            if k == eng and eng == "pe" and not dma:
                continue
            if s.seen[eng].get(k, 0) >= v:
                continue
            s.seen[eng][k] = v
            waits.append((k, v))
        if dma:
            n = s.NDMA[eng]
            key = "d%s%d" % (eng, s.drot[eng] % n)
            s.drot[eng] += 1
            s.dcnt[key] = s.dcnt.get(key, 0) + 16
            ev = (key, s.dcnt[key])
            inc = 16
        else:
            s.cnt[eng] += 1
            ev = (eng, s.cnt[eng])
            inc = 1
        s.ops[eng].append((waits, fn, ev[0], inc))
        for b in reads:
            if b.excl:
                b.last_w = ev
                b.readers = {}
            else:
                if b.readers.get(ev[0], 0) < ev[1]:
                    b.readers[ev[0]] = ev[1]
        for b in writes:
            b.last_w = ev
            b.readers = {}
        return ev

    def dma(s, out, in_, reads=(), writes=(), q="sp", **kw):
        return s.op(q, lambda e: e.dma_start(out, in_, **kw), reads, writes, dma=True)

    def sem_keys(s):
        keys = list(s.ENGS)
        for e in s.ENGS:
            for i in range(s.NDMA.get(e, 0)):
                keys.append("d%s%d" % (e, i))
        return keys

    def emit(s, final_waits=()):
        nc = s.nc
        keys = s.sem_keys()
        Pools.N[0] += 1
        sems = {k: nc.alloc_semaphore(name="%s_%d_%s" % (s.name, Pools.N[0], k)) for k in keys}
        with nc.Block() as block:

            def run(eng_name):
                def body(e):
                    for waits, fn, k, inc in s.ops[eng_name]:
                        for wk, wv in waits:
                            e.wait_ge(sems[wk], wv)
                        ins = fn(e)
                        ins.then_inc(sems[k], inc)
                    if eng_name == "sp":
                        for k, v in s.dcnt.items():
                            e.wait_ge(sems[k], v)
                return body

            block.tensor(run("pe"))
            block.scalar(run("act"))
            block.vector(run("dve"))
            block.gpsimd(run("pool"))
            block.sync(run("sp"))
        nc.clear_and_free_semaphores(list(sems.values()))
        nc.all_engine_barrier()


def clear_all_sems(nc):
    ph = Phase(nc, "init")
    sems = [nc.alloc_semaphore(name="init_%s" % k) for k in ph.sem_keys()]
    nc.clear_and_free_semaphores(sems)
    nc.all_engine_barrier()


class Pools:
    N = [0]

    def __init__(s, nc, st):
        s.nc, s.st = nc, st

    def sb(s, shape, dt, name=None):
        Pools.N[0] += 1
        t = s.st.enter_context(s.nc.sbuf_tensor("%s_%d" % (name or "sb", Pools.N[0]), list(shape), dt))
        return t, Buf(name or "sb")

    def ps(s, shape, dt, name=None):
        Pools.N[0] += 1
        t = s.st.enter_context(s.nc.psum_tensor("%s_%d" % (name or "ps", Pools.N[0]), list(shape), dt))
        return t, Buf(name or "ps", excl=True)


class Ring:
    def __init__(s, items):
        s.items = items
        s.i = 0

    def next(s):
        it = s.items[s.i % len(s.items)]
        s.i += 1
        return it


def bc_last(ap, n):
    shp = list(ap.shape)
    shp[-1] = n
    return ap.to_broadcast(shp)


def load_const(ph, P, dram_ap, shape, dt, name):
    t, b = P.sb(shape, dt, name)
    ph.dma(t[:], dram_ap, writes=[b])
    return t, b


def load_vecT(nc, ph, P, vec_ap, n, name):
    t, b = P.sb([128, n], F32, name)
    ph.dma(t[:], vec_ap.rearrange("(c p) -> p c", p=128), writes=[b], allow_slow_non_contiguous=True)
    return t, b


def build_hT(nc, cfg, ph, P, st, x_rows, ntok, gT, gTb, ident, identb, hT, hTb, res):
    D, DC = cfg.D, cfg.DC
    nt = ntok // 128
    for t in range(nt):
        xt, xb = res["xt"].next()
        ph.dma(xt[:], x_rows[t * 128:(t + 1) * 128, :], writes=[xb])
        xs, xsb = res["xs"].next()
        jk, jb = xs, xsb
        ss, ssb = res["ss"].next()
        ph.op("act", lambda e, xt=xt, jk=jk, ss=ss: e.activation(jk[:], xt[:], AF.Square, accum_out=ss[:, 0:1]),
              reads=[xb], writes=[jb, ssb])
        ph.op("act", lambda e, ss=ss: e.activation(ss[:, 1:2], ss[:, 0:1], AF.Sqrt, bias=res["eps"][:, 0:1], scale=1.0 / D),
              reads=[ssb, res["epsb"]], writes=[ssb])
        ph.op("dve", lambda e, ss=ss: e.reciprocal(ss[:, 2:3], ss[:, 1:2]), reads=[ssb], writes=[ssb])
        ph.op("dve", lambda e, xs=xs, xt=xt, ss=ss: e.tensor_scalar(xs[:], xt[:], ss[:, 2:3], None, ALU.mult),
              reads=[xb, ssb], writes=[xsb])
        for c0 in range(0, DC, 8):
            nch = min(8, DC - c0)
            tp, tpb = res["tp"].next()
            for k in range(nch):
                c = c0 + k
                ph.op("pe", lambda e, tp=tp, xs=xs, c=c, k=k: e.transpose(tp[:, k * 128:(k + 1) * 128], xs[:, c * 128:(c + 1) * 128], ident[:]),
                      reads=[xsb, identb], writes=[tpb])
            ph.op("dve", lambda e, tp=tp, c0=c0, nch=nch, t=t: e.tensor_tensor(
                hT[:, c0:c0 + nch, t * 128:(t + 1) * 128],
                tp[:, 0:nch * 128].rearrange("p (c n) -> p c n", n=128),
                gT[:, c0:c0 + nch].unsqueeze(2).to_broadcast([128, nch, 128]), ALU.mult),
                reads=[tpb, gTb], writes=[hTb])


def load_w_bf16(ph, res, w_cols, DC, ncols, wbf, wbfb, cast_rr):
    wv = w_cols.rearrange("(c p) n -> p c n", p=128)
    step = res["wst_chunks"]
    for c0 in range(0, DC, step):
        n = min(step, DC - c0)
        stg, stgb = res["wst"].next()
        ph.dma(stg[:, 0:n, 0:ncols], wv[:, c0:c0 + n, :], writes=[stgb])
        eng = cast_rr.next()
        if eng == "act":
            ph.op("act", lambda e, stg=stg, c0=c0, n=n: e.copy(wbf[:, c0:c0 + n, 0:ncols], stg[:, 0:n, 0:ncols]),
                  reads=[stgb], writes=[wbfb])
        else:
            ph.op(eng, lambda e, stg=stg, c0=c0, n=n: e.tensor_copy(wbf[:, c0:c0 + n, 0:ncols], stg[:, 0:n, 0:ncols]),
                  reads=[stgb], writes=[wbfb])


def phase_A(nc, cfg, io, scr):
    D, DC, T, TQ = cfg.D, cfg.DC, cfg.T, cfg.TQ
    W = cfg.HM * 128
    scale = 128 ** -0.5
    for (src, nrows, tag) in ((io["xb"], T, "kv"), (io["xq"], TQ, "q")):
        GT = min(1024, nrows)
        NS = GT // 512
        for g in range(nrows // GT):
            with ExitStack() as st:
                P = Pools(nc, st)
                ph = Phase(nc, "A%s%d" % (tag, g))
                ident, identb = load_const(ph, P, io["ident_bf"], [128, 128], BF16, "ident")
                ones, onesb = load_const(ph, P, io["ones_bf"], [128, 128], BF16, "ones")
                gT, gTb = load_vecT(nc, ph, P, io["norm_mix_g"], DC, "gT")
                gq, gqb = load_vecT(nc, ph, P, io["moba_q_norm_g"], 1, "gq")
                gk, gkb = load_vecT(nc, ph, P, io["moba_k_norm_g"], 1, "gk")
                eps, epsb = P.sb([128, 1], F32, "eps")
                ph.op("pool", lambda e: e.memset(eps[:], RMS_EPS), writes=[epsb])
                hT, hTb = P.sb([128, DC, GT], BF16, "hT")
                res = {
                    "xt": Ring([P.sb([128, D], F32, "xt") for _ in range(2)]),
                    "ss": Ring([P.sb([128, 4], F32, "ss") for _ in range(2)]),
                    "xs": Ring([P.sb([128, D], BF16, "xs") for _ in range(1)]),
                    "tp": Ring([P.ps([128, 1024], BF16, "tp") for _ in range(2)]),
                    "eps": eps, "epsb": epsb,
                    "wst_chunks": 16,
                    "wst": Ring([P.sb([128, 16, 256], F32, "wst") for _ in range(3)]),
                }
                for sub in range(NS):
                    build_hT(nc, cfg, ph, P, st, src[g * GT + sub * 512:g * GT + (sub + 1) * 512, :], 512, gT, gTb, ident, identb,
                             hT[:, :, sub * 512:(sub + 1) * 512], hTb, res)
                wring = Ring([P.sb([128, DC, 256], BF16, "wbf") for _ in range(2)])
                mm = Ring([P.ps([128, 512], F32, "mm") for _ in range(3)])
                sq_ps = Ring([P.ps([128, 512], F32, "sqp") for _ in range(2)])
                sqt = Ring([P.sb([128, 512], BF16, "sqt") for _ in range(2)])
                srt = Ring([P.sb([128, 512], F32, "srt") for _ in range(2)])
                obf = Ring([P.sb([128, 512], BF16, "obf") for _ in range(4)])
                vbf = Ring([P.sb([128, 4, 256], BF16, "vbf") for _ in range(2)])
                cast_rr = Ring(["act", "dve"])
                ev_rr = Ring(["act", "dve"])
                if tag == "kv":
                    jobs = []
                    for h0 in range(0, cfg.HM, 2):
                        jobs.append(("KM", W + h0 * 128, h0))
                        jobs.append(("VM", 2 * W + h0 * 128, h0))
                    for h0 in range(0, cfg.HS, 2):
                        jobs.append(("KS", 4 * W + h0 * 128, h0))
                        jobs.append(("VS", 5 * W + h0 * 128, h0))
                else:
                    jobs = []
                    for h0 in range(0, cfg.HM, 2):
                        jobs.append(("QM", 0 + h0 * 128, h0))
                    for h0 in range(0, cfg.HS, 2):
                        jobs.append(("QS", 3 * W + h0 * 128, h0))
                for (kind, col0, h0) in jobs:
                    wbf, wbfb = wring.next()
                    load_w_bf16(ph, res, io["w_in"][:, col0:col0 + 256], DC, 256, wbf, wbfb, cast_rr)
                    for sub in range(NS):
                      tsl = slice(g * GT + sub * 512, g * GT + (sub + 1) * 512)
                      hTs = hT[:, :, sub * 512:(sub + 1) * 512]
                      if kind in ("VM", "VS"):
                          vt, vtb = vbf.next()
                          for t in range(4):
                              ps, psb = mm.next()
                              for c in range(DC):
                                  ph.op("pe", lambda e, ps=ps, c=c, t=t, wbf=wbf, hTs=hTs: e.matmul(
                                      ps[:, 0:256], lhsT=hTs[:, c, t * 128:(t + 1) * 128], rhs=wbf[:, c, 0:256],
                                      start=(c == 0), stop=(c == DC - 1)), reads=[hTb, wbfb], writes=[psb])
                              eng = ev_rr.next()
                              if eng == "act":
                                  ph.op("act", lambda e, ps=ps, vt=vt, t=t: e.copy(vt[:, t, :], ps[:, 0:256]), reads=[psb], writes=[vtb])
                              else:
                                  ph.op("dve", lambda e, ps=ps, vt=vt, t=t: e.tensor_copy(vt[:, t, :], ps[:, 0:256]), reads=[psb], writes=[vtb])
                          dst = scr["vm"] if kind == "VM" else scr["vs"]
                          for hh in range(2):
                              ph.dma(dst[h0 + hh, tsl, :].rearrange("(t p) d -> p t d", p=128),
                                     vt[:, :, hh * 128:(hh + 1) * 128], reads=[vtb], q="pool")
                      else:
                          for ct in range(2):
                              h = h0 + ct
                              ps, psb = mm.next()
                              for c in range(DC):
                                  ph.op("pe", lambda e, ps=ps, c=c, ct=ct, wbf=wbf, hTs=hTs: e.matmul(
                                      ps[:], lhsT=wbf[:, c, ct * 128:(ct + 1) * 128], rhs=hTs[:, c, :],
                                      start=(c == 0), stop=(c == DC - 1)), reads=[hTb, wbfb], writes=[psb])
                              if kind in ("KM", "QM"):
                                  sq, sqb = sqt.next()
                                  ph.op("act", lambda e, sq=sq, ps=ps: e.activation(sq[:], ps[:], AF.Square), reads=[psb], writes=[sqb])
                                  sp_, spb = sq_ps.next()
                                  ph.op("pe", lambda e, sp_=sp_, sq=sq: e.matmul(sp_[:], lhsT=ones[:], rhs=sq[:], start=True, stop=True),
                                        reads=[sqb, onesb], writes=[spb])
                                  sr, srb = srt.next()
                                  ph.op("act", lambda e, sr=sr, sp_=sp_: e.activation(sr[:], sp_[:], AF.Sqrt, bias=eps[:, 0:1], scale=1.0 / 128),
                                        reads=[spb, epsb], writes=[srb])
                                  ph.op("dve", lambda e, sr=sr: e.reciprocal(sr[:], sr[:]), reads=[srb], writes=[srb])
                                  o, ob = obf.next()
                                  gg, ggb = (gk, gkb) if kind == "KM" else (gq, gqb)
                                  ph.op("dve", lambda e, o=o, ps=ps, sr=sr, gg=gg: e.scalar_tensor_tensor(
                                      o[:], ps[:], gg[:, 0:1], sr[:], ALU.mult, ALU.mult), reads=[psb, srb, ggb], writes=[ob])
                                  dst = scr["kTm"] if kind == "KM" else scr["qTm"]
                                  ph.dma(dst[h, :, tsl], o[:], reads=[ob], q="pool")
                              elif kind == "KS":
                                  o, ob = obf.next()
                                  ph.op("act", lambda e, o=o, ps=ps: e.copy(o[:], ps[:]), reads=[psb], writes=[ob])
                                  ph.dma(scr["kTs"][h, :, tsl], o[:], reads=[ob], q="pool")
                                  o2, ob2 = obf.next()
                                  ph.op("dve", lambda e, o2=o2, ps=ps: e.tensor_scalar(o2[:], ps[:], -scale, None, ALU.mult), reads=[psb], writes=[ob2])
                                  ph.dma(scr["nkTs"][h, :, tsl], o2[:], reads=[ob2], q="pool")
                              else:
                                  o, ob = obf.next()
                                  ph.op("act", lambda e, o=o, ps=ps: e.copy(o[:], ps[:]), reads=[psb], writes=[ob])
                                  ph.dma(scr["qTs"][h, :, tsl], o[:], reads=[ob], q="pool")
                ph.emit()


def declare_io(nc, cfg, debug=()):
    D, T, TQ = cfg.D, cfg.T, cfg.TQ
    W = cfg.HM * 128

    shapes = {}

    def inp(name, shape, dt=F32):
        shapes[name] = (list(shape), dt)
        return nc.dram_tensor(name, list(shape), dt, kind="ExternalInput").ap()

    io = {
        "xb": inp("xb", [T, D]), "xq": inp("xq", [TQ, D]), "memb": inp("memb", [cfg.NMEM, D]),
        "norm_mix_g": inp("norm_mix_g", [D]), "w_in": inp("w_in", [D, 3 * cfg.MIX]),
        "moba_q_norm_g": inp("moba_q_norm_g", [128]), "moba_k_norm_g": inp("moba_k_norm_g", [128]),
        "moba_out_norm_g": inp("moba_out_norm_g", [W]), "sb_out_norm_g": inp("sb_out_norm_g", [W]),
        "w_out": inp("w_out", [cfg.MIX, D]), "norm_xattn_g": inp("norm_xattn_g", [D]),
        "norm_mem_g": inp("norm_mem_g", [D]), "w_xq": inp("w_xq", [D, cfg.XW]),
        "w_xkv": inp("w_xkv", [D, 2 * cfg.XW]), "xattn_q_norm_g": inp("xattn_q_norm_g", [128]),
        "xattn_k_norm_g": inp("xattn_k_norm_g", [128]), "w_xo": inp("w_xo", [cfg.XW, D]),
        "norm_ffn_g": inp("norm_ffn_g", [D]), "w_peer_q": inp("w_peer_q", [D, cfg.PW]),
        "peer_sub_keys": inp("peer_sub_keys", [16, 128, 128]),
        "peer_u": inp("peer_u", [cfg.NE, D]), "peer_v": inp("peer_v", [cfg.NE, D]),
        "ident_bf": inp("ident_bf", [128, 128], BF16), "ones_bf": inp("ones_bf", [128, 128], BF16),
        "ident_f32": inp("ident_f32", [128, 128]),
        "tri_bf": inp("tri_bf", [128, 128], BF16),
        "moba_bias": inp("moba_bias", [cfg.HM, 128, cfg.NOWN * cfg.NB * 2]),
        "moba_alt": inp("moba_alt", [cfg.HM, 3, 256], BF16),
        "moba_sel": inp("moba_sel", [128, cfg.NB * 128], BF16),
        "gate_mask": inp("gate_mask", [128, cfg.NOWN * 2 * 16]),
        "cmask_moba": inp("cmask_moba", [128, 8 * 256], BF16),
        "cmask_sb": inp("cmask_sb", [128, 8 * 256], BF16),
    }
    io["y"] = nc.dram_tensor("y", [TQ, D], F32, kind="ExternalOutput").ap()
    io["_shapes"] = shapes

    def scratch(name, shape, dt):
        if name in debug:
            return nc.dram_tensor(name, list(shape), dt, kind="ExternalOutput").ap()
        return nc.dram_tensor(name, list(shape), dt).ap()

    scr = {
        "kTm": scratch("kTm", [cfg.HM, 128, T], BF16), "kTs": scratch("kTs", [cfg.HS, 128, T], BF16),
        "nkTs": scratch("nkTs", [cfg.HS, 128, T], BF16),
        "vm": scratch("vm", [cfg.HM, T, 128], BF16), "vs": scratch("vs", [cfg.HS, T, 128], BF16),
        "qTm": scratch("qTm", [cfg.HM, 128, TQ], BF16), "qTs": scratch("qTs", [cfg.HS, 128, TQ], BF16),
        "OT": scratch("OT", [cfg.HM + cfg.HS, 128, TQ], BF16),
        "x1": scratch("x1", [TQ, D], F32), "x2": scratch("x2", [TQ, D], F32),
        "kTx": scratch("kTx", [4, 128, cfg.NMEM], BF16), "vx": scratch("vx", [cfg.NMEM, 512], BF16),
        "uT": scratch("uT", [cfg.NE // 128, 128, cfg.DC, 128], BF16), "vbf": scratch("vbf", [cfg.NE, D], BF16),
        "hTf": scratch("hTf", [TQ // 256, 128, cfg.DC, 256], BF16),
        "sall": scratch("sall", [TQ // 128, 128, 8, 128], F32), "s1p": scratch("s1p", [TQ // 128, 128, 8, 128], F32),
        "thr": scratch("thr", [TQ // 128, 128, 8], F32),
    }
    return io, scr


def alibi_slopes_np(n):
    return (2.0 ** (-8.0 * np.arange(1, n + 1) / n)).astype(np.float64)


def split3_bf16(a):
    a = np.asarray(a, np.float64)
    hi = a.astype(ml_dtypes.bfloat16)
    r = a - hi.astype(np.float64)
    mid = r.astype(ml_dtypes.bfloat16)
    r2 = r - mid.astype(np.float64)
    lo = r2.astype(ml_dtypes.bfloat16)
    return hi, mid, lo


def host_consts(cfg, p):
    bf = ml_dtypes.bfloat16
    scale = 128 ** -0.5
    NB, NOWN, HM = cfg.NB, cfg.NOWN, cfg.HM
    own = own_blocks(cfg, p)
    c = {}
    c["ident_bf"] = np.eye(128, dtype=np.float32).astype(bf)
    c["ident_f32"] = np.eye(128, dtype=np.float32)
    c["ones_bf"] = np.ones((128, 128), np.float32).astype(bf)
    jj = np.arange(128)
    c["tri_bf"] = (jj[:, None] >= jj[None, :]).astype(np.float32).astype(bf)
    slopes = alibi_slopes_np(HM)
    mb = np.zeros((HM, 128, NOWN, NB, 2), np.float64)
    for i in range(NOWN):
        for n in range(NB):
            for kt in range(2):
                jabs = n * 256 + kt * 128 + jj
                val = jabs - own[i] * 256 - 128
                if n > own[i]:
                    mb[:, :, i, n, kt] = -BIG
                else:
                    mb[:, :, i, n, kt] = slopes[:, None] * val[None, :]
    c["moba_bias"] = mb.reshape(HM, 128, NOWN * NB * 2).astype(np.float32)
    tt = np.arange(256)
    alt = (-slopes[:, None] * (tt[None, :] - 128)) / scale
    hi, mid, lo = split3_bf16(alt)
    c["moba_alt"] = np.stack([hi, mid, lo], axis=1)
    sel = np.zeros((128, NB, 128), np.float32)
    for n in range(NB):
        sel[n, n, :] = 1.0
    sel[16:19, :, :] = 1.0
    c["moba_sel"] = sel.reshape(128, NB * 128).astype(bf)
    gm = np.zeros((128, NOWN, 2, 16), np.float32)
    for i in range(NOWN):
        for n in range(16):
            gm[:, i, 0, n] = 0.0 if n < own[i] else -1e30
            gm[:, i, 1, n] = BIG if n < own[i] else 0.0
    c["gate_mask"] = gm[:, :, :, :].reshape(128, NOWN * 2 * 16)
    def cm(strict):
        m = np.zeros((2, 2, 2, 128, 256), np.float32)
        for case in range(2):
            for slot in range(2):
                for kt in range(2):
                    jabs = slot * 256 + kt * 128 + jj
                    tabs = case * 256 + tt
                    if strict:
                        ok = jabs[:, None] < tabs[None, :]
                    else:
                        ok = jabs[:, None] <= tabs[None, :]
                    m[case, slot, kt] = ok
        return m
    def percore(m):
        out = np.zeros((128, 2, 2, 2, 256), np.float32)
        for ipar in range(2):
            case = p if ipar == 0 else 1 - p
            for slot in range(2):
                for kt in range(2):
                    out[:, ipar, slot, kt, :] = m[case, slot, kt]
        return out.reshape(128, 8 * 256)
    c["cmask_moba"] = ((percore(cm(False)) - 1.0) * BIG).astype(bf)
    c["cmask_sb"] = percore(cm(True)).astype(bf)
    return c


def f0_res(ph, P, io):
    R = {}
    R["ident"], R["identb"] = load_const(ph, P, io["ident_bf"], [128, 128], BF16, "f0ident")
    return R


def f0_alloc(P, cfg, R):
    D, DC = cfg.D, cfg.DC
    R["ut"] = Ring([P.sb([128, D], F32, "f0ut") for _ in range(2)])
    R["ub"] = Ring([P.sb([128, D], BF16, "f0ub") for _ in range(2)])
    R["vt"] = Ring([P.sb([128, D], F32, "f0vt") for _ in range(2)])
    R["vb"] = Ring([P.sb([128, D], BF16, "f0vb") for _ in range(2)])
    R["uTs"] = Ring([P.sb([128, DC, 128], BF16, "f0uTs") for _ in range(2)])
    R["tp"] = Ring([P.ps([128, 1024], BF16, "f0tp") for _ in range(1)])
    R["st"] = {}


def f0_stageA(ph, cfg, io, scr, R, i):
    rows = slice(i * 128, (i + 1) * 128)
    u_, u_b = R["ut"].next()
    ph.dma(u_[:], io["peer_u"][rows, :], writes=[u_b])
    ubf, ubfb = R["ub"].next()
    ph.op("act", lambda e: e.copy(ubf[:], u_[:]), reads=[u_b], writes=[ubfb])
    R["st"][i] = (ubf, ubfb)
    v_, v_b = R["vt"].next()
    ph.dma(v_[:], io["peer_v"][rows, :], writes=[v_b])
    vbf, vbfb = R["vb"].next()
    ph.op("dve", lambda e: e.tensor_copy(vbf[:], v_[:]), reads=[v_b], writes=[vbfb])
    ph.dma(scr["vbf"][rows, :], vbf[:], reads=[vbfb], q="pool")


def f0_stageB(ph, cfg, io, scr, R, i):
    DC = cfg.DC
    ubf, ubfb = R["st"].pop(i)
    ident, identb = R["ident"], R["identb"]
    uo, uob = R["uTs"].next()
    for c0 in range(0, DC, 8):
        nch = min(8, DC - c0)
        t_, t_b = R["tp"].next()
        for k in range(nch):
            c = c0 + k
            ph.op("pe", lambda e, t_=t_, c=c, k=k: e.transpose(t_[:, k * 128:(k + 1) * 128], ubf[:, c * 128:(c + 1) * 128], ident[:]),
                  reads=[ubfb, identb], writes=[t_b])
        ph.op("dve", lambda e, t_=t_, c0=c0, nch=nch: e.tensor_copy(uo[:, c0:c0 + nch, :], t_[:, 0:nch * 128].rearrange("p (c n) -> p c n", n=128)),
              reads=[t_b], writes=[uob])
    ph.dma(scr["uT"][i], uo[:], reads=[uob], q="pool")


class F0Sched:
    def __init__(s, cfg):
        s.cfg = cfg
        s.next_chunk = 0
        s.nheads = cfg.HM
        s.nch = cfg.NE // 128
        s.per_head = -(-s.nch // s.nheads)

    def begin_head(s):
        n = min(s.per_head, s.nch - s.next_chunk)
        s.cur = list(range(s.next_chunk, s.next_chunk + n))
        s.next_chunk += n
        s.a_done = 0
        s.b_done = 0

    def tick(s, ph, io, scr, R, frac):
        n = len(s.cur)
        if n == 0:
            return
        ta = min(n, int(frac * (n + 1)) + 1)
        tb = n if frac >= 1.0 else max(0, ta - 1)
        while s.a_done < ta or s.b_done < tb:
            if s.a_done < ta and s.a_done - s.b_done < 2:
                f0_stageA(ph, s.cfg, io, scr, R, s.cur[s.a_done])
                s.a_done += 1
            elif s.b_done < s.a_done:
                f0_stageB(ph, s.cfg, io, scr, R, s.cur[s.b_done])
                s.b_done += 1
            else:
                break

    def finish(s, ph, io, scr, R):
        s.tick(ph, io, scr, R, 2.0)


def phase_B(nc, cfg, io, scr, f0=None):
    T, TQ, NB, NOWN = cfg.T, cfg.TQ, cfg.NB, cfg.NOWN
    KT = T // 128
    scale = 128 ** -0.5
    for h in range(cfg.HM):
        with ExitStack() as st:
            P = Pools(nc, st)
            ph = Phase(nc, "B%d" % h)
            if f0 is not None:
                R0 = f0_res(ph, P, io)
                f0_alloc(P, cfg, R0)
                f0.begin_head()
            identf, identfb = load_const(ph, P, io["ident_f32"], [128, 128], F32, "identf")
            ones, onesb = load_const(ph, P, io["ones_bf"], [128, 128], BF16, "ones")
            sel, selb = load_const(ph, P, io["moba_sel"], [128, NB * 128], BF16, "sel")
            gmask, gmaskb = load_const(ph, P, io["gate_mask"], [128, NOWN * 2 * 16], F32, "gmask")
            cmask, cmaskb = load_const(ph, P, io["cmask_moba"], [128, 8 * 256], BF16, "cmask")
            bias, biasb = load_const(ph, P, io["moba_bias"][h], [128, NOWN * NB * 2], F32, "bias")
            kT, kTb = load_const(ph, P, scr["kTm"][h], [128, T], BF16, "kT")
            qT, qTb = load_const(ph, P, scr["qTm"][h], [128, TQ], BF16, "qT")
            vv, vvb = P.sb([128, KT, 128], BF16, "vv")
            ph.dma(vv[:], scr["vm"][h].rearrange("(k p) d -> p k d", p=128), writes=[vvb])
            mrhs_all, mrhsb = P.sb([128, NOWN, 256], BF16, "mrhs")
            ph.op("pool", lambda e: e.memset(mrhs_all[:], 0.0), writes=[mrhsb])
            for i in range(NOWN):
                ph.dma(mrhs_all[16:19, i, :], io["moba_alt"][h], writes=[mrhsb])
            kmf, kmfb = P.sb([128, 16], F32, "kmf")
            km, kmb = P.sb([128, 16], BF16, "km")
            ph.op("dve", lambda e: e.tensor_reduce(kmf[:, 0:NB], kT[:].rearrange("p (n j) -> p n j", j=256), AX.X, ALU.add),
                  reads=[kTb], writes=[kmfb])
            ph.op("dve", lambda e: e.tensor_scalar(km[:, 0:NB], kmf[:, 0:NB], 1.0 / 256, None, ALU.mult), reads=[kmfb], writes=[kmb])
            gmr = Ring([P.sb([128, 16], F32, "gm") for _ in range(2)])
            for (gm_, gmb_) in gmr.items:
                ph.op("pool", lambda e, gm_=gm_: e.memset(gm_[:], -3.0e38), writes=[gmb_])
            m8r = Ring([P.sb([128, 8], F32, "m8") for _ in range(2)])
            svr = Ring([P.sb([128, 16], F32, "selv") for _ in range(2)])
            ngr = Ring([P.sb([128, 16], F32, "negm") for _ in range(2)])
            gpr = Ring([P.ps([128, 512], F32, "gps") for _ in range(1)])
            tpr = gpr
            sps = Ring([P.ps([128, 512], F32, "sps") for _ in range(2)])
            ops_ = Ring([P.ps([128, 512], F32, "ops") for _ in range(2)])
            dps = Ring([P.ps([128, 512], F32, "dps") for _ in range(2)])
            pbf = Ring([P.sb([128, 256], BF16, "pbf") for _ in range(4)])
            tmp = Ring([P.sb([128, 256], F32, "tmp") for _ in range(2)])
            rd, rdb = P.sb([128, 256], F32, "rden")
            obf = Ring([P.sb([128, 256], BF16, "obf") for _ in range(2)])

            def gate(i, qt):
                gps, gpsb = gpr.next()
                tps, tpsb = tpr.next()
                gm, gmb = gmr.next()
                m8, m8b = m8r.next()
                sv_, svb = svr.next()
                ng, ngb = ngr.next()
                ph.op("pe", lambda e: e.matmul(gps[:, 0:NB], lhsT=qT[:, i * 256 + qt * 128:i * 256 + (qt + 1) * 128],
                                               rhs=km[:, 0:NB], start=True, stop=True), reads=[qTb, kmb], writes=[gpsb])
                g0 = (i * 2 + 0) * 16
                g1 = (i * 2 + 1) * 16
                ph.op("dve", lambda e: e.tensor_tensor(gm[:, 0:NB], gps[:, 0:NB], gmask[:, g0:g0 + NB], ALU.add), reads=[gpsb, gmaskb], writes=[gmb])
                ph.op("dve", lambda e: e.max(m8[:], gm[:]), reads=[gmb], writes=[m8b])
                ph.op("dve", lambda e: e.scalar_tensor_tensor(sv_[:], gm[:], m8[:, 2:3], gmask[:, g1:g1 + 16], ALU.is_ge, ALU.mult),
                      reads=[gmb, m8b, gmaskb], writes=[svb])
                ph.op("dve", lambda e: e.tensor_tensor(ng[:], sv_[:], gmask[:, g1:g1 + 16], ALU.subtract), reads=[svb, gmaskb], writes=[ngb])
                ph.op("pe", lambda e: e.transpose(tps[0:16, 128:256], ng[:], identf[:]), reads=[ngb, identfb], writes=[tpsb])
                ph.op("act", lambda e: e.copy(mrhs_all[0:16, i, qt * 128:(qt + 1) * 128], tps[0:16, 128:256]), reads=[tpsb], writes=[mrhsb])

            for i in range(NOWN):
                for qt in range(2):
                    gate(i, qt)
            tiles = []
            for i in range(NOWN):
                nkt = (2 * i + 2) * 2
                for kti in range(nkt):
                    tiles.append({"i": i, "kti": kti, "first": kti == 0, "last": kti == nkt - 1})
            blk = {}

            def s1(tl):
                i, kti = tl["i"], tl["kti"]
                qs = slice(i * 256, (i + 1) * 256)
                n, kt = kti // 2, kti % 2
                S, Sb = sps.next()
                ph.op("pe", lambda e: e.matmul(S[:, 0:256], lhsT=kT[:, kti * 128:(kti + 1) * 128], rhs=qT[:, qs], start=True, stop=False),
                      reads=[kTb, qTb], writes=[Sb])
                ph.op("pe", lambda e: e.matmul(S[:, 0:256], lhsT=sel[:, n * 128:(n + 1) * 128], rhs=mrhs_all[:, i, :], start=False, stop=True),
                      reads=[selb, mrhsb], writes=[Sb])
                bcol = (i * NB + n) * 2 + kt
                Pt, Ptb = pbf.next()
                if n >= 2 * i:
                    cidx = ((i % 2) * 2 + (n - 2 * i)) * 2 + kt
                    tm, tmb = tmp.next()
                    ph.op("dve", lambda e: e.tensor_tensor(tm[:], S[:, 0:256], cmask[:, cidx * 256:(cidx + 1) * 256], ALU.add), reads=[Sb, cmaskb], writes=[tmb])
                    ph.op("act", lambda e: e.activation(Pt[:], tm[:], AF.Exp, bias=bias[:, bcol:bcol + 1], scale=scale), reads=[tmb, biasb], writes=[Ptb])
                else:
                    ph.op("act", lambda e: e.activation(Pt[:], S[:, 0:256], AF.Exp, bias=bias[:, bcol:bcol + 1], scale=scale), reads=[Sb, biasb], writes=[Ptb])
                tl["P"], tl["Pb"] = Pt, Ptb

            def s2(tl):
                i, kti = tl["i"], tl["kti"]
                qs = slice(i * 256, (i + 1) * 256)
                if tl["first"]:
                    blk[i] = ops_.next() + dps.next()
                O, Ob, Dn, Dnb = blk[i]
                Pt, Ptb = tl["P"], tl["Pb"]
                ph.op("pe", lambda e: e.matmul(O[:, 0:256], lhsT=vv[:, kti, :], rhs=Pt[:], start=tl["first"], stop=tl["last"]), reads=[vvb, Ptb], writes=[Ob])
                ph.op("pe", lambda e: e.matmul(Dn[:, 0:256], lhsT=ones[:], rhs=Pt[:], start=tl["first"], stop=tl["last"]), reads=[onesb, Ptb], writes=[Dnb])
                if tl["last"]:
                    ph.op("dve", lambda e: e.reciprocal(rd[:], Dn[:, 0:256]), reads=[Dnb], writes=[rdb])
                    o, ob = obf.next()
                    ph.op("dve", lambda e: e.tensor_tensor(o[:], O[:, 0:256], rd[:], ALU.mult), reads=[Ob, rdb], writes=[ob])
                    ph.dma(scr["OT"][h, :, qs], o[:], reads=[ob], q="pool")

            NT_ = len(tiles)
            for k in range(NT_ + 2):
                if f0 is not None:
                    f0.tick(ph, io, scr, R0, k / float(NT_ + 2))
                if k < NT_:
                    s1(tiles[k])
                if 0 <= k - 2 < NT_:
                    s2(tiles[k - 2])
            if f0 is not None:
                f0.finish(ph, io, scr, R0)
            ph.emit()


def phase_C(nc, cfg, io, scr, f0=None):
    T, TQ, NB, NOWN = cfg.T, cfg.TQ, cfg.NB, cfg.NOWN
    KT = T // 128
    scale = 128 ** -0.5
    for h in range(cfg.HS):
        with ExitStack() as st:
            P = Pools(nc, st)
            ph = Phase(nc, "C%d" % h)
            if f0 is not None:
                R0 = f0_res(ph, P, io)
                f0_alloc(P, cfg, R0)
                f0.begin_head()
            ones, onesb = load_const(ph, P, io["ones_bf"], [128, 128], BF16, "ones")
            tri, trib = load_const(ph, P, io["tri_bf"], [128, 128], BF16, "tri")
            cmask, cmaskb = load_const(ph, P, io["cmask_sb"], [128, 8 * 256], BF16, "cmask")
            kT, kTb = load_const(ph, P, scr["kTs"][h], [128, T], BF16, "kT")
            nkT, nkTb = load_const(ph, P, scr["nkTs"][h], [128, T], BF16, "nkT")
            qT, qTb = load_const(ph, P, scr["qTs"][h], [128, TQ], BF16, "qT")
            vv, vvb = P.sb([128, KT, 128], BF16, "vv")
            ph.dma(vv[:], scr["vs"][h].rearrange("(k p) d -> p k d", p=128), writes=[vvb])
            one1, one1b = P.sb([128, 1], F32, "one1")
            ph.op("pool", lambda e: e.memset(one1[:], 1.0), writes=[one1b])
            zps = Ring([P.ps([128, 512], F32, "zps") for _ in range(3)])
            cps = Ring([P.ps([128, 512], F32, "cps") for _ in range(2)])
            ops_ = Ring([P.ps([128, 512], F32, "ops") for _ in range(2)])
            Ef = Ring([P.sb([128, 512], F32, "Ef") for _ in range(3)])
            Lb = Ring([P.sb([128, 512], BF16, "Lb") for _ in range(5)])
            ab = Ring([P.sb([128, 512], BF16, "ab") for _ in range(4)])
            Ls = Ring([P.sb([128, 256], BF16, "Lsum") for _ in range(2)])
            obf = Ring([P.sb([128, 256], BF16, "obf") for _ in range(2)])
            tiles = []
            for i in range(NOWN):
                npair = 2 * i + 2
                for m in range(npair - 1, -1, -1):
                    tiles.append({"i": i, "m": m, "first": m == npair - 1, "last": m == 0})
            blk = {}

            def s1(tl):
                i, m = tl["i"], tl["m"]
                qs = slice(i * 256, (i + 1) * 256)
                if tl["first"]:
                    O, Ob = ops_.next()
                    Lsum, Lsumb = Ls.next()
                    ph.op("dve", lambda e: e.memset(Lsum[:], 0.0), writes=[Lsumb])
                    blk[i] = (O, Ob, Lsum, Lsumb)
                Z, Zb = zps.next()
                for hf in range(2):
                    kti = 2 * m + hf
                    ph.op("pe", lambda e, hf=hf, kti=kti: e.matmul(Z[:, hf * 256:(hf + 1) * 256], lhsT=kT[:, kti * 128:(kti + 1) * 128], rhs=qT[:, qs], start=True, stop=True),
                          reads=[kTb, qTb], writes=[Zb])
                E, Eb = Ef.next()
                ph.op("act", lambda e: e.activation(E[:], Z[:], AF.Exp, scale=scale), reads=[Zb], writes=[Eb])
                L, Lbb = Lb.next()
                ph.op("act", lambda e: e.activation(L[:], E[:], AF.Ln, bias=one1[:, 0:1], scale=1.0), reads=[Eb, one1b], writes=[Lbb])
                c0 = None
                if m >= 2 * i:
                    c0 = ((i % 2) * 2 + (m - 2 * i)) * 2 * 256
                    ph.op("dve", lambda e: e.tensor_tensor(L[:], L[:], cmask[:, c0:c0 + 512], ALU.mult), reads=[cmaskb], writes=[Lbb])
                tl["L"], tl["Lbb"], tl["c0"] = L, Lbb, c0

            def s2a(tl):
                i, m = tl["i"], tl["m"]
                qs = slice(i * 256, (i + 1) * 256)
                O, Ob, Lsum, Lsumb = blk[i]
                L, Lbb, c0 = tl["L"], tl["Lbb"], tl["c0"]
                C, Cb = cps.next()
                for hf in (1, 0):
                    kti = 2 * m + hf
                    cs = slice(hf * 256, (hf + 1) * 256)
                    ph.op("pe", lambda e, kti=kti, cs=cs: e.matmul(C[:, cs], lhsT=nkT[:, kti * 128:(kti + 1) * 128], rhs=qT[:, qs], start=True, stop=False),
                          reads=[nkTb, qTb], writes=[Cb])
                    ph.op("pe", lambda e, cs=cs: e.matmul(C[:, cs], lhsT=tri[:], rhs=L[:, cs], start=False, stop=False), reads=[trib, Lbb], writes=[Cb])
                    ph.op("pe", lambda e, cs=cs, hf=hf: e.matmul(C[:, cs], lhsT=ones[:], rhs=Lsum[:], start=False, stop=(hf == 1)), reads=[onesb, Lsumb], writes=[Cb])
                    if hf == 0:
                        ph.op("pe", lambda e, cs=cs: e.matmul(C[:, cs], lhsT=ones[:], rhs=L[:, 256:512], start=False, stop=True), reads=[onesb, Lbb], writes=[Cb])
                if not tl["last"]:
                    ph.op("dve", lambda e: e.tensor_tensor(Lsum[:], Lsum[:], L[:, 0:256], ALU.add), reads=[Lbb], writes=[Lsumb])
                    ph.op("dve", lambda e: e.tensor_tensor(Lsum[:], Lsum[:], L[:, 256:512], ALU.add), reads=[Lbb], writes=[Lsumb])
                a_, abb = ab.next()
                ph.op("act", lambda e: e.activation(a_[:], C[:], AF.Exp, scale=-1.0), reads=[Cb], writes=[abb])
                if c0 is not None:
                    ph.op("dve", lambda e: e.tensor_tensor(a_[:], a_[:], cmask[:, c0:c0 + 512], ALU.mult), reads=[cmaskb], writes=[abb])
                tl["a"], tl["abb"] = a_, abb

            def s2b(tl):
                i, m = tl["i"], tl["m"]
                qs = slice(i * 256, (i + 1) * 256)
                O, Ob, Lsum, Lsumb = blk[i]
                a_, abb = tl["a"], tl["abb"]
                for hf in (1, 0):
                    kti = 2 * m + hf
                    ph.op("pe", lambda e, kti=kti, hf=hf: e.matmul(O[:, 0:256], lhsT=vv[:, kti, :], rhs=a_[:, hf * 256:(hf + 1) * 256],
                                                                  start=(tl["first"] and hf == 1), stop=(tl["last"] and hf == 0)), reads=[vvb, abb], writes=[Ob])
                if tl["last"]:
                    o, ob = obf.next()
                    ph.op("dve", lambda e: e.tensor_copy(o[:], O[:, 0:256]), reads=[Ob], writes=[ob])
                    ph.dma(scr["OT"][cfg.HM + h, :, qs], o[:], reads=[ob], q="pool")

            NT_ = len(tiles)
            for k in range(NT_ + 3):
                if f0 is not None:
                    f0.tick(ph, io, scr, R0, k / float(NT_ + 3))
                if k < NT_:
                    s1(tiles[k])
                if 0 <= k - 1 < NT_:
                    s2a(tiles[k - 1])
                if 0 <= k - 2 < NT_:
                    s2b(tiles[k - 2])
            if f0 is not None:
                f0.finish(ph, io, scr, R0)
            ph.emit()


def mk_norm_res(P):
    return {
        "sq_ps": Ring([P.ps([128, 512], F32, "sqp") for _ in range(2)]),
        "sqt": Ring([P.sb([128, 512], BF16, "sqt") for _ in range(2)]),
        "srt": Ring([P.sb([128, 512], F32, "srt") for _ in range(2)]),
    }


def qknorm_evac(ph, ps, psb, n, gg, ggb, ones, onesb, eps, epsb, nr, o, ob):
    sq, sqb = nr["sqt"].next()
    ph.op("act", lambda e: e.activation(sq[:, 0:n], ps[:, 0:n], AF.Square), reads=[psb], writes=[sqb])
    sp_, spb = nr["sq_ps"].next()
    ph.op("pe", lambda e: e.matmul(sp_[:, 0:n], lhsT=ones[:], rhs=sq[:, 0:n], start=True, stop=True), reads=[sqb, onesb], writes=[spb])
    sr, srb = nr["srt"].next()
    ph.op("act", lambda e: e.activation(sr[:, 0:n], sp_[:, 0:n], AF.Sqrt, bias=eps[:, 0:1], scale=1.0 / 128), reads=[spb, epsb], writes=[srb])
    ph.op("dve", lambda e: e.reciprocal(sr[:, 0:n], sr[:, 0:n]), reads=[srb], writes=[srb])
    ph.op("dve", lambda e: e.scalar_tensor_tensor(o, ps[:, 0:n], gg[:, 0:1], sr[:, 0:n], ALU.mult, ALU.mult), reads=[psb, srb, ggb], writes=[ob])


def hT_res(P, D, nxt=2, nxs=2):
    return {
        "xt": Ring([P.sb([128, D], F32, "xt") for _ in range(nxt)]),
        "ss": Ring([P.sb([128, 4], F32, "ss") for _ in range(2)]),
        "xs": Ring([P.sb([128, D], BF16, "xs") for _ in range(nxs)]),
        "tp": Ring([P.ps([128, 1024], BF16, "tp") for _ in range(2)]),
        "wst_chunks": 16,
        "wst": Ring([P.sb([128, 16, 256], F32, "wst") for _ in range(2)]),
    }


def std_consts(nc, ph, P, io):
    c = {}
    c["ident"], c["identb"] = load_const(ph, P, io["ident_bf"], [128, 128], BF16, "ident")
    c["ones"], c["onesb"] = load_const(ph, P, io["ones_bf"], [128, 128], BF16, "ones")
    c["eps"], c["epsb"] = P.sb([128, 1], F32, "eps")
    ph.op("pool", lambda e: e.memset(c["eps"][:], RMS_EPS), writes=[c["epsb"]])
    return c


def phase_D(nc, cfg, io, scr):
    D, DC, TQ = cfg.D, cfg.DC, cfg.TQ
    W = cfg.HM * 128
    NH = cfg.HM + cfg.HS
    GT = min(1024, TQ)
    for g in range(TQ // GT):
        with ExitStack() as st:
            P = Pools(nc, st)
            ph = Phase(nc, "D%d" % g)
            k = std_consts(nc, ph, P, io)
            ones, onesb, eps, epsb = k["ones"], k["onesb"], k["eps"], k["epsb"]
            gout, goutb = P.sb([128, NH], F32, "gout")
            ph.dma(gout[:, 0:cfg.HM], io["moba_out_norm_g"].rearrange("(c p) -> p c", p=128), writes=[goutb], allow_slow_non_contiguous=True)
            ph.dma(gout[:, cfg.HM:NH], io["sb_out_norm_g"].rearrange("(c p) -> p c", p=128), writes=[goutb], allow_slow_non_contiguous=True)
            tsl = slice(g * GT, (g + 1) * GT)
            OTs, OTsb = P.sb([128, NH, GT], BF16, "OTs")
            ph.dma(OTs[:], scr["OT"][:, :, tsl].rearrange("h p t -> p h t"), writes=[OTsb])
            sqt = Ring([P.sb([128, 512], BF16, "sqt") for _ in range(2)])
            ssq = Ring([P.ps([128, 512], F32, "ssq") for _ in range(2)])
            rs = Ring([P.sb([128, 512], F32, "rs") for _ in range(2)])
            for sub in range(GT // 512):
                ss_ = slice(sub * 512, (sub + 1) * 512)
                for (h0, h1) in ((0, cfg.HM), (cfg.HM, NH)):
                    sp_, spb = ssq.next()
                    for h in range(h0, h1):
                        sq, sqb = sqt.next()
                        ph.op("act", lambda e, sq=sq, h=h, ss_=ss_: e.activation(sq[:], OTs[:, h, ss_], AF.Square), reads=[OTsb], writes=[sqb])
                        ph.op("pe", lambda e, sq=sq, sp_=sp_, h=h, h0=h0, h1=h1: e.matmul(sp_[:], lhsT=ones[:], rhs=sq[:], start=(h == h0), stop=(h == h1 - 1)),
                              reads=[sqb, onesb], writes=[spb])
                    r, rb = rs.next()
                    ph.op("act", lambda e, r=r, sp_=sp_: e.activation(r[:], sp_[:], AF.Sqrt, bias=eps[:, 0:1], scale=1.0 / W), reads=[spb, epsb], writes=[rb])
                    ph.op("dve", lambda e, r=r: e.reciprocal(r[:], r[:]), reads=[rb], writes=[rb])
                    for h in range(h0, h1):
                        ph.op("dve", lambda e, r=r, h=h, ss_=ss_: e.scalar_tensor_tensor(OTs[:, h, ss_], OTs[:, h, ss_], gout[:, h:h + 1], r[:], ALU.mult, ALU.mult),
                              reads=[rb, goutb], writes=[OTsb])
            res = {"wst_chunks": 16, "wst": Ring([P.sb([128, 16, 256], F32, "wst") for _ in range(4)])}
            wring = Ring([P.sb([128, DC, 256], BF16, "wbf") for _ in range(3)])
            mm = Ring([P.ps([128, 512], F32, "mm") for _ in range(3)])
            xr = Ring([P.sb([128, 256], F32, "xr") for _ in range(3)])
            cast_rr = Ring(["act", "dve"])
            for j in range(D // 256):
                wbf, wbfb = wring.next()
                load_w_bf16(ph, res, io["w_out"][:, j * 256:(j + 1) * 256], DC, 256, wbf, wbfb, cast_rr)
                for t in range(GT // 128):
                    rows = slice(g * GT + t * 128, g * GT + (t + 1) * 128)
                    x_, xb_ = xr.next()
                    ph.dma(x_[:], io["xq"][rows, j * 256:(j + 1) * 256], writes=[xb_])
                    ps, psb = mm.next()
                    for c in range(DC):
                        ph.op("pe", lambda e, ps=ps, c=c, t=t, wbf=wbf: e.matmul(ps[:, 0:256], lhsT=OTs[:, c, t * 128:(t + 1) * 128], rhs=wbf[:, c, :],
                                                                              start=(c == 0), stop=(c == DC - 1)), reads=[OTsb, wbfb], writes=[psb])
                    ph.op("dve", lambda e, x_=x_, ps=ps: e.tensor_tensor(x_[:], ps[:, 0:256], x_[:], ALU.add), reads=[psb], writes=[xb_])
                    ph.dma(scr["x1"][rows, j * 256:(j + 1) * 256], x_[:], reads=[xb_], q="pool")
            ph.emit()


def phase_E(nc, cfg, io, scr):
    D, DC, TQ, NM = cfg.D, cfg.DC, cfg.TQ, cfg.NMEM
    scale = 128 ** -0.5
    with ExitStack() as st:
        P = Pools(nc, st)
        ph = Phase(nc, "E0")
        k = std_consts(nc, ph, P, io)
        ones, onesb, eps, epsb = k["ones"], k["onesb"], k["eps"], k["epsb"]
        gT, gTb = load_vecT(nc, ph, P, io["norm_mem_g"], DC, "gT")
        gk, gkb = load_vecT(nc, ph, P, io["xattn_k_norm_g"], 1, "gk")
        res = hT_res(P, D)
        res["eps"], res["epsb"] = eps, epsb
        mT, mTb = P.sb([128, DC, NM], BF16, "mT")
        build_hT(nc, cfg, ph, P, st, io["memb"], NM, gT, gTb, k["ident"], k["identb"], mT, mTb, res)
        nr = mk_norm_res(P)
        wring = Ring([P.sb([128, DC, 256], BF16, "wbf") for _ in range(2)])
        mm = Ring([P.ps([128, 512], F32, "mm") for _ in range(2)])
        obf = Ring([P.sb([128, NM], BF16, "obf") for _ in range(2)])
        vbf = Ring([P.sb([128, 256], BF16, "vbf") for _ in range(2)])
        cast_rr = Ring(["act", "dve"])
        for j in range(4):
            wbf, wbfb = wring.next()
            load_w_bf16(ph, res, io["w_xkv"][:, j * 256:(j + 1) * 256], DC, 256, wbf, wbfb, cast_rr)
            if j < 2:
                for ct in range(2):
                    h = j * 2 + ct
                    ps, psb = mm.next()
                    for c in range(DC):
                        ph.op("pe", lambda e, ps=ps, c=c, ct=ct, wbf=wbf: e.matmul(ps[:, 0:NM], lhsT=wbf[:, c, ct * 128:(ct + 1) * 128], rhs=mT[:, c, :],
                                                                                 start=(c == 0), stop=(c == DC - 1)), reads=[mTb, wbfb], writes=[psb])
                    o, ob = obf.next()
                    qknorm_evac(ph, ps, psb, NM, gk, gkb, ones, onesb, eps, epsb, nr, o[:], ob)
                    ph.dma(scr["kTx"][h], o[:], reads=[ob], q="pool")
            else:
                for t in range(NM // 128):
                    ps, psb = mm.next()
                    for c in range(DC):
                        ph.op("pe", lambda e, ps=ps, c=c, t=t, wbf=wbf: e.matmul(ps[:, 0:256], lhsT=mT[:, c, t * 128:(t + 1) * 128], rhs=wbf[:, c, :],
                                                                              start=(c == 0), stop=(c == DC - 1)), reads=[mTb, wbfb], writes=[psb])
                    v_, vb_ = vbf.next()
                    ph.op("act", lambda e, v_=v_, ps=ps: e.copy(v_[:], ps[:, 0:256]), reads=[psb], writes=[vb_])
                    ph.dma(scr["vx"][t * 128:(t + 1) * 128, (j - 2) * 256:(j - 1) * 256], v_[:], reads=[vb_], q="pool")
        ph.emit()
    GT = 512
    MT = NM // 128
    for g in range(TQ // GT):
        with ExitStack() as st:
            P = Pools(nc, st)
            ph = Phase(nc, "E1_%d" % g)
            k = std_consts(nc, ph, P, io)
            ones, onesb, eps, epsb = k["ones"], k["onesb"], k["eps"], k["epsb"]
            gT, gTb = load_vecT(nc, ph, P, io["norm_xattn_g"], DC, "gT")
            gq, gqb = load_vecT(nc, ph, P, io["xattn_q_norm_g"], 1, "gq")
            kTx, kTxb = P.sb([128, 4, NM], BF16, "kTx")
            ph.dma(kTx[:], scr["kTx"].rearrange("h p m -> p h m"), writes=[kTxb])
            vx, vxb = P.sb([128, MT, 512], BF16, "vx")
            ph.dma(vx[:], scr["vx"].rearrange("(t p) c -> p t c", p=128), writes=[vxb])
            res = hT_res(P, D, nxt=1)
            res["eps"], res["epsb"] = eps, epsb
            hT, hTb = P.sb([128, DC, GT], BF16, "hT")
            build_hT(nc, cfg, ph, P, st, scr["x1"][g * GT:(g + 1) * GT, :], GT, gT, gTb, k["ident"], k["identb"], hT, hTb, res)
            nr = mk_norm_res(P)
            wring = Ring([P.sb([128, DC, 256], BF16, "wbf") for _ in range(2)])
            mm = Ring([P.ps([128, 512], F32, "mm") for _ in range(2)])
            qTx, qTxb = P.sb([128, 4, GT], BF16, "qTx")
            cast_rr = Ring(["act", "dve"])
            for j in range(2):
                wbf, wbfb = wring.next()
                load_w_bf16(ph, res, io["w_xq"][:, j * 256:(j + 1) * 256], DC, 256, wbf, wbfb, cast_rr)
                for ct in range(2):
                    h = j * 2 + ct
                    ps, psb = mm.next()
                    for c in range(DC):
                        ph.op("pe", lambda e, ps=ps, c=c, ct=ct, wbf=wbf: e.matmul(ps[:, 0:GT], lhsT=wbf[:, c, ct * 128:(ct + 1) * 128], rhs=hT[:, c, :],
                                                                                 start=(c == 0), stop=(c == DC - 1)), reads=[hTb, wbfb], writes=[psb])
                    qknorm_evac(ph, ps, psb, GT, gq, gqb, ones, onesb, eps, epsb, nr, qTx[:, h, :], qTxb)
            OTx, OTxb = P.sb([128, 4, GT], BF16, "OTx")
            pbf = Ring([P.sb([128, GT], BF16, "pbf") for _ in range(2)])
            rd, rdb = P.sb([128, GT], F32, "rd")
            ops_, opsb = P.ps([128, 512], F32, "ops")
            dps, dpsb = P.ps([128, 512], F32, "dps")
            for h in range(4):
                for m in range(MT):
                    S, Sb = mm.next()
                    ph.op("pe", lambda e, S=S, h=h, m=m: e.matmul(S[:, 0:GT], lhsT=kTx[:, h, m * 128:(m + 1) * 128], rhs=qTx[:, h, :], start=True, stop=True),
                          reads=[kTxb, qTxb], writes=[Sb])
                    Pt, Ptb = pbf.next()
                    ph.op("act", lambda e, Pt=Pt, S=S: e.activation(Pt[:], S[:, 0:GT], AF.Exp, scale=scale), reads=[Sb], writes=[Ptb])
                    ph.op("pe", lambda e, Pt=Pt, h=h, m=m: e.matmul(ops_[:, 0:GT], lhsT=vx[:, m, h * 128:(h + 1) * 128], rhs=Pt[:], start=(m == 0), stop=(m == MT - 1)),
                          reads=[vxb, Ptb], writes=[opsb])
                    ph.op("pe", lambda e, Pt=Pt, m=m: e.matmul(dps[:, 0:GT], lhsT=ones[:], rhs=Pt[:], start=(m == 0), stop=(m == MT - 1)),
                          reads=[onesb, Ptb], writes=[dpsb])
                ph.op("dve", lambda e: e.reciprocal(rd[:], dps[:, 0:GT]), reads=[dpsb], writes=[rdb])
                ph.op("dve", lambda e, h=h: e.tensor_tensor(OTx[:, h, :], ops_[:, 0:GT], rd[:], ALU.mult), reads=[opsb, rdb], writes=[OTxb])
            wxo = Ring([P.sb([128, 4, 256], BF16, "wxo") for _ in range(2)])
            wxs = Ring([P.sb([128, 4, 256], F32, "wxs") for _ in range(2)])
            xr = Ring([P.sb([128, 256], F32, "xr") for _ in range(3)])
            for j in range(D // 256):
                ws, wsb = wxs.next()
                ph.dma(ws[:], io["w_xo"][:, j * 256:(j + 1) * 256].rearrange("(c p) n -> p c n", p=128), writes=[wsb])
                wb, wbb = wxo.next()
                ph.op("act", lambda e, wb=wb, ws=ws: e.copy(wb[:], ws[:]), reads=[wsb], writes=[wbb])
                for t in range(GT // 128):
                    rows = slice(g * GT + t * 128, g * GT + (t + 1) * 128)
                    x_, xb_ = xr.next()
                    ph.dma(x_[:], scr["x1"][rows, j * 256:(j + 1) * 256], writes=[xb_])
                    ps, psb = mm.next()
                    for h in range(4):
                        ph.op("pe", lambda e, ps=ps, h=h, t=t, wb=wb: e.matmul(ps[:, 0:256], lhsT=OTx[:, h, t * 128:(t + 1) * 128], rhs=wb[:, h, :],
                                                                            start=(h == 0), stop=(h == 3)), reads=[OTxb, wbb], writes=[psb])
                    ph.op("dve", lambda e, x_=x_, ps=ps: e.tensor_tensor(x_[:], ps[:, 0:256], x_[:], ALU.add), reads=[psb], writes=[xb_])
                    ph.dma(scr["x2"][rows, j * 256:(j + 1) * 256], x_[:], reads=[xb_], q="pool")
            ph.emit()


def phase_F0(nc, cfg, io, scr):
    D, DC = cfg.D, cfg.DC
    NCH = cfg.NE // 128
    CPP = 16
    for g in range(NCH // CPP):
        with ExitStack() as st:
            P = Pools(nc, st)
            ph = Phase(nc, "F0_%d" % g)
            ident, identb = load_const(ph, P, io["ident_bf"], [128, 128], BF16, "ident")
            ut = Ring([P.sb([128, D], F32, "ut") for _ in range(2)])
            ub = Ring([P.sb([128, D], BF16, "ub") for _ in range(2)])
            vt = Ring([P.sb([128, D], F32, "vt") for _ in range(2)])
            vb = Ring([P.sb([128, D], BF16, "vb") for _ in range(2)])
            uTs = Ring([P.sb([128, DC, 128], BF16, "uTs") for _ in range(2)])
            tp = Ring([P.ps([128, 1024], BF16, "tp") for _ in range(3)])
            ev = Ring(["dve", "act"])
            for i in range(g * CPP, (g + 1) * CPP):
                rows = slice(i * 128, (i + 1) * 128)
                u_, u_b = ut.next()
                ph.dma(u_[:], io["peer_u"][rows, :], writes=[u_b])
                ubf, ubfb = ub.next()
                ph.op("act", lambda e, ubf=ubf, u_=u_: e.copy(ubf[:], u_[:]), reads=[u_b], writes=[ubfb])
                uo, uob = uTs.next()
                for c0 in range(0, DC, 8):
                    nch = min(8, DC - c0)
                    t_, t_b = tp.next()
                    for k in range(nch):
                        c = c0 + k
                        ph.op("pe", lambda e, t_=t_, ubf=ubf, c=c, k=k: e.transpose(t_[:, k * 128:(k + 1) * 128], ubf[:, c * 128:(c + 1) * 128], ident[:]),
                              reads=[ubfb, identb], writes=[t_b])
                    eng = ev.next()
                    if eng == "act":
                        ph.op("act", lambda e, uo=uo, t_=t_, c0=c0, nch=nch: e.copy(uo[:, c0:c0 + nch, :], t_[:, 0:nch * 128].rearrange("p (c n) -> p c n", n=128)),
                              reads=[t_b], writes=[uob])
                    else:
                        ph.op("dve", lambda e, uo=uo, t_=t_, c0=c0, nch=nch: e.tensor_copy(uo[:, c0:c0 + nch, :], t_[:, 0:nch * 128].rearrange("p (c n) -> p c n", n=128)),
                              reads=[t_b], writes=[uob])
                ph.dma(scr["uT"][i], uo[:], reads=[uob], q="pool")
                v_, v_b = vt.next()
                ph.dma(v_[:], io["peer_v"][rows, :], writes=[v_b])
                vbf, vbfb = vb.next()
                ph.op("dve", lambda e, vbf=vbf, v_=v_: e.tensor_copy(vbf[:], v_[:]), reads=[v_b], writes=[vbfb])
                ph.dma(scr["vbf"][rows, :], vbf[:], reads=[vbfb], q="pool")
            ph.emit()


def phase_F1a(nc, cfg, io, scr):
    D, DC, TQ = cfg.D, cfg.DC, cfg.TQ
    GT = min(512, TQ)
    NTL = GT // 128
    NSL = 4
    for g in range(TQ // GT):
        with ExitStack() as st:
            P = Pools(nc, st)
            ph = Phase(nc, "F1a_%d" % g)
            k = std_consts(nc, ph, P, io)
            eps, epsb = k["eps"], k["epsb"]
            identf, identfb = load_const(ph, P, io["ident_f32"], [128, 128], F32, "identf")
            gT, gTb = load_vecT(nc, ph, P, io["norm_ffn_g"], DC, "gT")
            res = hT_res(P, D, nxt=1, nxs=1)
            res["eps"], res["epsb"] = eps, epsb
            hT, hTb = P.sb([128, DC, GT], BF16, "hT")
            build_hT(nc, cfg, ph, P, st, scr["x2"][g * GT:(g + 1) * GT, :], GT, gT, gTb, k["ident"], k["identb"], hT, hTb, res)
            for hf in range(GT // 256):
                ph.dma(scr["hTf"][g * (GT // 256) + hf], hT[:, :, hf * 256:(hf + 1) * 256], reads=[hTb], q="pool")
            skf, skfb = P.sb([128, 16, 128], F32, "skf")
            ph.dma(skf[:], io["peer_sub_keys"].rearrange("a k c -> k a c"), writes=[skfb])
            skT, skTb = P.sb([128, 16, 128], BF16, "skT")
            mm = Ring([P.ps([128, 512], F32, "mm") for _ in range(2)])
            sps = Ring([P.ps([128, 512], F32, "sps") for _ in range(2)])
            for a0 in range(0, 16, 4):
                ps, psb = mm.next()
                for a in range(4):
                    ph.op("pe", lambda e, ps=ps, a=a, a0=a0: e.transpose(ps[:, a * 128:(a + 1) * 128], skf[:, a0 + a, :], identf[:]),
                          reads=[skfb, identfb], writes=[psb])
                ph.op("dve", lambda e, ps=ps, a0=a0: e.tensor_copy(skT[:, a0:a0 + 4, :], ps[:].rearrange("p (a k) -> p a k", k=128)),
                      reads=[psb], writes=[skTb])
            wring = Ring([P.sb([128, DC, 256], BF16, "wbf") for _ in range(2)])
            cast_rr = Ring(["act", "dve"])
            qTr = Ring([P.sb([128, GT], BF16, "qT") for _ in range(2)])
            sall = [P.sb([128, 16, 128], F32, "sall") for _ in range(NTL)]
            for j in range(8):
                wbf, wbfb = wring.next()
                load_w_bf16(ph, res, io["w_peer_q"][:, j * 256:(j + 1) * 256], DC, 256, wbf, wbfb, cast_rr)
                for ct in range(2):
                    hp = 2 * j + ct
                    ps, psb = mm.next()
                    for c in range(DC):
                        ph.op("pe", lambda e, ps=ps, c=c, ct=ct, wbf=wbf: e.matmul(ps[:, 0:GT], lhsT=wbf[:, c, ct * 128:(ct + 1) * 128], rhs=hT[:, c, :],
                                                                                 start=(c == 0), stop=(c == DC - 1)), reads=[hTb, wbfb], writes=[psb])
                    q_, q_b = qTr.next()
                    ph.op("act", lambda e, q_=q_, ps=ps: e.copy(q_[:], ps[:, 0:GT]), reads=[psb], writes=[q_b])
                    for t in range(NTL):
                        s_, s_b = sps.next()
                        ph.op("pe", lambda e, s_=s_, q_=q_, t=t, hp=hp: e.matmul(s_[:, 0:128], lhsT=q_[:, t * 128:(t + 1) * 128], rhs=skT[:, hp, :], start=True, stop=True),
                              reads=[q_b, skTb], writes=[s_b])
                        ph.op("dve", lambda e, s_=s_, t=t, hp=hp: e.tensor_copy(sall[t][0][:, hp, :], s_[:, 0:128]), reads=[s_b], writes=[sall[t][1]])
            slots = []
            for _ in range(NSL):
                slots.append({
                    "v01": P.sb([128, 2, 16], F32, "v01"), "wk": [P.sb([128, 128], F32, "wk") for _ in range(2)],
                    "cand": P.sb([128, 16, 16], F32, "cand"), "c24": P.sb([128, 24], F32, "c24"),
                    "w2": P.sb([128, 256], F32, "w2"), "w3": P.sb([128, 256], F32, "w3"),
                    "sc": P.sb([128, 8], F32, "sc"), "e16": P.sb([128, 16], F32, "e16"),
                })
            s1pp = [P.sb([128, 8, 128], F32, "s1pp") for _ in range(NTL)]
            thrr = [P.sb([128, 8], F32, "thr") for _ in range(NTL)]
            kapr = [P.sb([128, 8], F32, "kap") for _ in range(NTL)]

            def chain(t, h, S):
                sa, sab = sall[t]
                v01, v01b = S["v01"]
                cand, candb = S["cand"]
                c24, c24b = S["c24"]
                w2, w2b = S["w2"]
                w3, w3b = S["w3"]
                sc, scb = S["sc"]
                e16, e16b = S["e16"]
                thr, thrb = thrr[t]
                steps = []
                for pp in range(2):
                    wk, wkb = S["wk"][pp]
                    sx = sa[:, 2 * h + pp, :]
                    steps.append(lambda sx=sx, pp=pp: ph.op("dve", lambda e: e.max(v01[:, pp, 0:8], sx), reads=[sab], writes=[v01b]))
                    steps.append(lambda sx=sx, pp=pp, wk=wk, wkb=wkb: ph.op("dve", lambda e: e.match_replace(wk[:], v01[:, pp, 0:8], sx, -1.0e30), reads=[sab, v01b], writes=[wkb]))
                    steps.append(lambda pp=pp, wk=wk, wkb=wkb: ph.op("dve", lambda e: e.max(v01[:, pp, 8:16], wk[:]), reads=[wkb], writes=[v01b]))
                cf = cand[:].rearrange("p a b -> p (a b)")
                steps.append(lambda: ph.op("dve", lambda e: e.tensor_tensor(cand[:], v01[:, 0, :].unsqueeze(2).to_broadcast([128, 16, 16]),
                                                                           v01[:, 1, :].unsqueeze(1).to_broadcast([128, 16, 16]), ALU.add), reads=[v01b], writes=[candb]))
                steps.append(lambda: ph.op("dve", lambda e: e.max(c24[:, 0:8], cf), reads=[candb], writes=[c24b]))
                steps.append(lambda: ph.op("dve", lambda e: e.match_replace(w2[:], c24[:, 0:8], cf, -1.0e30), reads=[candb, c24b], writes=[w2b]))
                steps.append(lambda: ph.op("dve", lambda e: e.max(c24[:, 8:16], w2[:]), reads=[w2b], writes=[c24b]))
                steps.append(lambda: ph.op("dve", lambda e: e.match_replace(w3[:], c24[:, 8:16], w2[:], -1.0e30), reads=[w2b, c24b], writes=[w3b]))
                steps.append(lambda: ph.op("dve", lambda e: e.max(c24[:, 16:24], w3[:]), reads=[w3b], writes=[c24b]))
                steps.append(lambda: ph.op("dve", lambda e: e.tensor_scalar(sc[:, 0:1], c24[:, 0:1], -1.0, None, ALU.mult), reads=[c24b], writes=[scb]))
                steps.append(lambda: ph.op("act", lambda e: e.activation(e16[:], c24[:, 0:16], AF.Exp, bias=sc[:, 0:1], scale=1.0, accum_out=sc[:, 1:2]),
                                           reads=[c24b, scb], writes=[e16b, scb]))
                steps.append(lambda: ph.op("act", lambda e: e.activation(sc[:, 2:3], sc[:, 1:2], AF.Ln), reads=[scb], writes=[scb]))
                steps.append(lambda: ph.op("dve", lambda e: e.tensor_tensor(sc[:, 3:4], sc[:, 0:1], sc[:, 2:3], ALU.subtract), reads=[scb], writes=[scb]))
                steps.append(lambda: ph.op("dve", lambda e: e.tensor_tensor(sc[:, 4:5], c24[:, 15:16], c24[:, 16:17], ALU.add), reads=[scb, c24b], writes=[scb]))
                steps.append(lambda: ph.op("dve", lambda e: e.tensor_scalar(sc[:, 5:6], sc[:, 4:5], 0.5, None, ALU.mult), reads=[scb], writes=[scb]))
                steps.append(lambda: ph.op("dve", lambda e: e.tensor_tensor(thr[:, h:h + 1], sc[:, 5:6], sc[:, 3:4], ALU.add), reads=[scb], writes=[thrb]))
                steps.append(lambda: ph.op("dve", lambda e: e.tensor_scalar(s1pp[t][0][:, h, :], sa[:, 2 * h + 1, :], sc[:, 5:6], None, ALU.subtract),
                                           reads=[scb, sab], writes=[s1pp[t][1]]))
                return steps

            jobs = [(t, h) for t in range(NTL) for h in range(8)]
            for j0 in range(0, len(jobs), NSL):
                chains = [chain(t, h, slots[si]) for si, (t, h) in enumerate(jobs[j0:j0 + NSL])]
                for step in range(max(len(c) for c in chains)):
                    for c in chains:
                        if step < len(c):
                            c[step]()
            for t in range(NTL):
                ti = g * NTL + t
                ph.op("act", lambda e, t=t: e.activation(kapr[t][0][:], thrr[t][0][:], AF.Exp), reads=[thrr[t][1]], writes=[kapr[t][1]])
                ph.dma(scr["sall"][ti], sall[t][0][:].rearrange("p (h two) k -> p h two k", two=2)[:, :, 0, :], reads=[sall[t][1]], q="pool")
                ph.dma(scr["s1p"][ti], s1pp[t][0][:], reads=[s1pp[t][1]], q="pool")
                ph.dma(scr["thr"][ti], kapr[t][0][:], reads=[kapr[t][1]], q="pool")
            ph.emit()


def phase_F1b(nc, cfg, io, scr):
    D, DC, TQ = cfg.D, cfg.DC, cfg.TQ
    GT = 256
    NCH = cfg.NE // 128
    bw = min(512, D // 2)
    bpr = 2 if D >= 2048 else 1
    npair = 2
    nbk = bpr * npair
    rw = bpr * bw
    rounds_total = 2 * (D // rw)
    rounds_per_iter = (rounds_total + 3) // 4
    for g in range(TQ // GT):
        with ExitStack() as st:
            P = Pools(nc, st)
            ph = Phase(nc, "F1b_%d" % g)
            identb_, identbb = load_const(ph, P, io["ident_bf"], [128, 128], BF16, "identb")
            hT, hTb = load_const(ph, P, scr["hTf"][g], [128, DC, GT], BF16, "hT")
            sall, s1p, thr, osb = [], [], [], []
            for t in range(2):
                ti = g * 2 + t
                sall.append(load_const(ph, P, scr["sall"][ti], [128, 8, 128], F32, "sall"))
                s1p.append(load_const(ph, P, scr["s1p"][ti], [128, 8, 128], F32, "s1p"))
                thr.append(load_const(ph, P, scr["thr"][ti], [128, 8], F32, "thr"))
                osb.append(load_const(ph, P, scr["x2"][ti * 128:(ti + 1) * 128, :], [128, D], F32, "osb"))
            kdiag, kdiagb = P.sb([128, 2, 8, 128], BF16, "kdiag")
            for t in range(2):
                for h in range(8):
                    ph.op("dve", lambda e, t=t, h=h: e.tensor_scalar(kdiag[:, t, h, :], identb_[:], thr[t][0][:, h:h + 1], None, ALU.mult),
                          reads=[identbb, thr[t][1]], writes=[kdiagb])
            uTr = Ring([P.sb([128, DC, 128], BF16, "uT") for _ in range(2)])
            vbr = Ring([P.sb([128, D], BF16, "vb") for _ in range(9)])
            aps = Ring([P.ps([128, 512], F32, "aps") for _ in range(2)])
            gpr = Ring([P.ps([128, 512], F32, "gps") for _ in range(2)])
            ops_ = [P.ps([128, 512], F32, "ops") for _ in range(nbk)]
            actr = Ring([P.sb([128, GT], BF16, "actT") for _ in range(3)])
            Dr = Ring([P.sb([128, 8, 128], F32, "Dt") for _ in range(2)])
            Er = Ring([P.sb([128, 8, 128], F32, "Et") for _ in range(2)])
            Fr = Ring([P.sb([128, 8, 128], BF16, "Ft") for _ in range(10)])
            GAr = Ring([P.sb([128, 4, GT], BF16, "GA") for _ in range(2)])
            st_ = {}

            def gate(i):
                Gs = []
                for t in range(2):
                    sa, sab = sall[t]
                    Dt, Dtb = Dr.next()
                    s0i = sa[:, :, i:i + 1].to_broadcast([128, 8, 128])
                    ph.op("pool", lambda e, Dt=Dt, s0i=s0i, t=t: e.tensor_tensor(Dt[:], s1p[t][0][:], s0i, ALU.add), reads=[s1p[t][1], sab], writes=[Dtb])
                    Et, Etb = Er.next()
                    ph.op("act", lambda e, Et=Et, Dt=Dt: e.activation(Et[:], Dt[:], AF.Exp), reads=[Dtb], writes=[Etb])
                    Ft, Ftb = Fr.next()
                    ph.op("dve", lambda e, Ft=Ft, Dt=Dt, Et=Et: e.scalar_tensor_tensor(Ft[:].rearrange("p h j -> p (h j)"), Dt[:].rearrange("p h j -> p (h j)"), 0.0,
                                                                                     Et[:].rearrange("p h j -> p (h j)"), ALU.is_ge, ALU.mult),
                          reads=[Dtb, Etb], writes=[Ftb])
                    Gs.append((Ft, Ftb))
                st_[("G", i)] = Gs

            def s1(i):
                uT, uTb = uTr.next()
                ph.dma(uT[:], scr["uT"][i], writes=[uTb])
                vb, vbb = vbr.next()
                ph.dma(vb[:], scr["vbf"][i * 128:(i + 1) * 128, :], writes=[vbb])
                st_[("vb", i)] = (vb, vbb)
                A, Ab = aps.next()
                for c in range(DC):
                    ph.op("pe", lambda e, c=c: e.matmul(A[:, 0:GT], lhsT=uT[:, c, :], rhs=hT[:, c, :], start=(c == 0), stop=(c == DC - 1)),
                          reads=[uTb, hTb], writes=[Ab])
                aT, aTb = actr.next()
                ph.op("act", lambda e: e.activation(aT[:], A[:, 0:GT], AF.Gelu), reads=[Ab], writes=[aTb])
                st_[("a", i)] = (aT, aTb)

            def s2(i):
                c4 = i % 4
                if c4 == 0:
                    st_["GA"] = GAr.next()
                GA, GAb = st_["GA"]
                aT, aTb = st_.pop(("a", i))
                Gs = st_.pop(("G", i))
                for t in range(2):
                    G, Gb = Gs[t]
                    gps, gpsb = gpr.next()
                    for h in range(8):
                        ph.op("pe", lambda e, G=G, gps=gps, h=h, t=t: e.matmul(gps[:, 0:128], lhsT=G[:, h, :], rhs=kdiag[:, t, h, :], start=(h == 0), stop=(h == 7)),
                              reads=[Gb, kdiagb], writes=[gpsb])
                    ph.op("dve", lambda e, t=t, gps=gps: e.tensor_tensor(GA[:, c4, t * 128:(t + 1) * 128], gps[:, 0:128], aT[:, t * 128:(t + 1) * 128], ALU.mult),
                          reads=[gpsb, aTb], writes=[GAb])

            pending = []
            add_q = []

            def push_s3(cg):
                GA, GAb = st_["GA"]
                vbs = [st_.pop(("vb", cg * 4 + c4)) for c4 in range(4)]
                for t in range(2):
                    for col0 in range(0, D, rw):
                        pending.append((GA, GAb, vbs, t, col0))

            rr_pair = [0]

            def emit_round():
                GA, GAb, vbs, t, col0 = pending.pop(0)
                pair = rr_pair[0] % npair
                rr_pair[0] += 1
                banks = ops_[pair * bpr:(pair + 1) * bpr]
                for c4 in range(4):
                    vb, vbb = vbs[c4]
                    for b in range(bpr):
                        cc = col0 + b * bw
                        ph.op("pe", lambda e, b=b, c4=c4, vb=vb, cc=cc: e.matmul(banks[b][0][:, 0:bw], lhsT=GA[:, c4, t * 128:(t + 1) * 128], rhs=vb[:, cc:cc + bw],
                                                                            start=(c4 == 0), stop=(c4 == 3)), reads=[GAb, vbb], writes=[banks[b][1]])
                flush_adds()
                for b in range(bpr):
                    cc = col0 + b * bw
                    add_q.append((banks[b], t, cc))

            def flush_adds():
                while add_q:
                    (bk, bkb), t, cc = add_q.pop(0)
                    ph.op("dve", lambda e, bk=bk, t=t, cc=cc: e.tensor_tensor(osb[t][0][:, cc:cc + bw], bk[:, 0:bw], osb[t][0][:, cc:cc + bw], ALU.add),
                          reads=[bkb], writes=[osb[t][1]])

            for k in range(NCH + 4):
                if k < NCH:
                    gate(k)
                if 0 <= k - 1 < NCH:
                    s1(k - 1)
                if 0 <= k - 3 < NCH:
                    s2(k - 3)
                    if (k - 3) % 4 == 3:
                        push_s3((k - 3) // 4)
                for _ in range(rounds_per_iter):
                    if pending:
                        emit_round()
            while pending:
                emit_round()
            flush_adds()
            for t in range(2):
                ti = g * 2 + t
                ph.dma(io["y"][ti * 128:(ti + 1) * 128, :], osb[t][0][:], reads=[osb[t][1]], q="pool")
            ph.emit()


def build_program(cfg, debug=()):
    nc = bass.Bass("TRN2", target_bir_lowering=False)
    io, scr = declare_io(nc, cfg, debug=debug)
    clear_all_sems(nc)
    phase_A(nc, cfg, io, scr)
    f0 = F0Sched(cfg)
    phase_B(nc, cfg, io, scr, f0)
    assert f0.next_chunk == cfg.NE // 128
    phase_C(nc, cfg, io, scr, None)
    phase_D(nc, cfg, io, scr)
    phase_E(nc, cfg, io, scr)
    phase_F1a(nc, cfg, io, scr)
    phase_F1b(nc, cfg, io, scr)
    return nc, io


def kernel(x, mem, norm_mix_g, w_in, moba_q_norm_g, moba_k_norm_g, moba_out_norm_g, sb_out_norm_g, w_out,
           norm_xattn_g, norm_mem_g, w_xq, w_xkv, xattn_q_norm_g, xattn_k_norm_g, w_xo, norm_ffn_g,
           w_peer_q, peer_sub_keys, peer_u, peer_v):
    x = np.asarray(x, np.float32)
    B, T, D = x.shape
    cfg = Cfg(D=D, T=T, B=B, NMEM=np.asarray(mem).shape[1])
    nc, io = build_program(cfg)

    def f(a):
        return np.ascontiguousarray(np.asarray(a, np.float32)[0])

    shared = {
        "norm_mix_g": f(norm_mix_g), "w_in": f(w_in), "moba_q_norm_g": f(moba_q_norm_g), "moba_k_norm_g": f(moba_k_norm_g),
        "moba_out_norm_g": f(moba_out_norm_g), "sb_out_norm_g": f(sb_out_norm_g), "w_out": f(w_out),
        "norm_xattn_g": f(norm_xattn_g), "norm_mem_g": f(norm_mem_g), "w_xq": f(w_xq), "w_xkv": f(w_xkv),
        "xattn_q_norm_g": f(xattn_q_norm_g), "xattn_k_norm_g": f(xattn_k_norm_g), "w_xo": f(w_xo),
        "norm_ffn_g": f(norm_ffn_g), "w_peer_q": f(w_peer_q),
        "peer_sub_keys": f(peer_sub_keys).reshape(16, 128, 128), "peer_u": f(peer_u), "peer_v": f(peer_v),
    }
    consts = [host_consts(cfg, p) for p in range(2)]
    mem = np.asarray(mem, np.float32)
    in_maps = []
    rows_of = []
    for core in range(cfg.ncores):
        b, p = core // 2, core % 2
        rows = np.concatenate([np.arange(blk * 256, (blk + 1) * 256) for blk in own_blocks(cfg, p)])
        rows_of.append((b, rows))
        m = dict(shared)
        m["xb"] = np.ascontiguousarray(x[b])
        m["xq"] = np.ascontiguousarray(x[b][rows])
        m["memb"] = np.ascontiguousarray(mem[b])
        for k, v in consts[p].items():
            if k in io["_shapes"]:
                m[k] = v
        in_maps.append(m)
    res = run_bass_kernel_spmd(nc, in_maps, core_ids=list(range(cfg.ncores)))
    out = np.empty((B, T, D), np.float32)
    for core in range(cfg.ncores):
        b, rows = rows_of[core]
        out[b, rows] = np.asarray(res.results[core]["y"], np.float32)
    return out
```

```python
import math
from contextlib import ExitStack
import numpy as np
import ml_dtypes
import concourse.bass as bass
import concourse.mybir as mybir
from concourse.bass_utils import run_bass_kernel_spmd

F32 = mybir.dt.float32
BF16 = mybir.dt.bfloat16
AF = mybir.ActivationFunctionType
ALU = mybir.AluOpType
AX = mybir.AxisListType

RMS_EPS = 1e-6
BIG = 30000.0


class Cfg:
    def __init__(s, D=4096, T=4096, B=4, NMEM=256):
        s.D, s.T, s.B, s.NMEM = D, T, B, NMEM
        s.DC = D // 128
        s.HM = D // 256
        s.HS = D // 256
        s.MIX = D
        s.NB = T // 256
        s.NOWN = s.NB // 2
        s.TQ = T // 2
        s.XW = 512
        s.PH = 8
        s.NK = 128
        s.NE = 128 * 128
        s.PW = 8 * 256
        s.ncores = 2 * B


def own_blocks(cfg, p):
    out = []
    for i in range(cfg.NOWN):
        a, b = 2 * i, 2 * i + 1
        if i % 2 == 0:
            out.append(a if p == 0 else b)
        else:
            out.append(b if p == 0 else a)
    return out


class Buf:
    __slots__ = ("name", "last_w", "readers", "excl")

    def __init__(s, name, excl=False):
        s.name = name
        s.last_w = None
        s.readers = {}
        s.excl = excl


class Phase:
    ENGS = ("pe", "act", "dve", "pool", "sp")
    NDMA = {"sp": 8, "pool": 4, "act": 2}

    def __init__(s, nc, name):
        s.nc = nc
        s.name = name
        s.ops = {e: [] for e in s.ENGS}
        s.cnt = {e: 0 for e in s.ENGS}
        s.seen = {e: {} for e in s.ENGS}
        s.dcnt = {}
        s.drot = {e: 0 for e in s.ENGS}

    def op(s, eng, fn, reads=(), writes=(), dma=False):
        deps = {}

        def add(ev):
            if ev is None:
                return
            k, v = ev
            if deps.get(k, 0) < v:
                deps[k] = v

        for b in reads:
            add(b.last_w)
            if b.excl:
                for k, v in b.readers.items():
                    add((k, v))
        for b in writes:
            add(b.last_w)
            for k, v in b.readers.items():
                add((k, v))
        waits = []
        for k, v in deps.items():
            if k == eng and eng == "pe" and not dma:
                continue
            if s.seen[eng].get(k, 0) >= v:
                continue
            s.seen[eng][k] = v
            waits.append((k, v))
        if dma:
            n = s.NDMA[eng]
            key = "d%s%d" % (eng, s.drot[eng] % n)
            s.drot[eng] += 1
            s.dcnt[key] = s.dcnt.get(key, 0) + 16
            ev = (key, s.dcnt[key])
            inc = 16
        else:
            s.cnt[eng] += 1
            ev = (eng, s.cnt[eng])
            inc = 1
        s.ops[eng].append((waits, fn, ev[0], inc))
        for b in reads:
            if b.excl:
                b.last_w = ev
                b.readers = {}
            else:
                if b.readers.get(ev[0], 0) < ev[1]:
                    b.readers[ev[0]] = ev[1]
        for b in writes:
            b.last_w = ev
            b.readers = {}
        return ev

    def dma(s, out, in_, reads=(), writes=(), q="sp", **kw):
        return s.op(q, lambda e: e.dma_start(out, in_, **kw), reads, writes, dma=True)

    def sem_keys(s):
        keys = list(s.ENGS)
        for e in s.ENGS:
            for i in range(s.NDMA.get(e, 0)):
                keys.append("d%s%d" % (e, i))
        return keys

    def emit(s, final_waits=()):
        nc = s.nc
        keys = s.sem_keys()
        Pools.N[0] += 1
        sems = {k: nc.alloc_semaphore(name="%s_%d_%s" % (s.name, Pools.N[0], k)) for k in keys}
        with nc.Block() as block:

            def run(eng_name):
                def body(e):
                    for waits, fn, k, inc in s.ops[eng_name]:
                        for wk, wv in waits:
                            e.wait_ge(sems[wk], wv)
                        ins = fn(e)
                        ins.then_inc(sems[k], inc)
                    if eng_name == "sp":
                        for k, v in s.dcnt.items():
                            e.wait_ge(sems[k], v)
                return body

            block.tensor(run("pe"))
            block.scalar(run("act"))
            block.vector(run("dve"))
            block.gpsimd(run("pool"))
            block.sync(run("sp"))
        nc.clear_and_free_semaphores(list(sems.values()))
        nc.all_engine_barrier()


def clear_all_sems(nc):
    ph = Phase(nc, "init")
    sems = [nc.alloc_semaphore(name="init_%s" % k) for k in ph.sem_keys()]
    nc.clear_and_free_semaphores(sems)
    nc.all_engine_barrier()


class Pools:
    N = [0]

    def __init__(s, nc, st):
        s.nc, s.st = nc, st

    def sb(s, shape, dt, name=None):
        Pools.N[0] += 1
        t = s.st.enter_context(s.nc.sbuf_tensor("%s_%d" % (name or "sb", Pools.N[0]), list(shape), dt))
        return t, Buf(name or "sb")

    def ps(s, shape, dt, name=None):
        Pools.N[0] += 1
        t = s.st.enter_context(s.nc.psum_tensor("%s_%d" % (name or "ps", Pools.N[0]), list(shape), dt))
        return t, Buf(name or "ps", excl=True)


class Ring:
    def __init__(s, items):
        s.items = items
        s.i = 0

    def next(s):
        it = s.items[s.i % len(s.items)]
        s.i += 1
        return it


def bc_last(ap, n):
    shp = list(ap.shape)
    shp[-1] = n
    return ap.to_broadcast(shp)


def load_const(ph, P, dram_ap, shape, dt, name):
    t, b = P.sb(shape, dt, name)
    ph.dma(t[:], dram_ap, writes=[b])
    return t, b


def load_vecT(nc, ph, P, vec_ap, n, name):
    t, b = P.sb([128, n], F32, name)
    ph.dma(t[:], vec_ap.rearrange("(c p) -> p c", p=128), writes=[b], allow_slow_non_contiguous=True)
    return t, b


def build_hT(nc, cfg, ph, P, st, x_rows, ntok, gT, gTb, ident, identb, hT, hTb, res):
    D, DC = cfg.D, cfg.DC
    nt = ntok // 128
    for t in range(nt):
        xt, xb = res["xt"].next()
        ph.dma(xt[:], x_rows[t * 128:(t + 1) * 128, :], writes=[xb])
        xs, xsb = res["xs"].next()
        jk, jb = xs, xsb
        ss, ssb = res["ss"].next()
        ph.op("act", lambda e, xt=xt, jk=jk, ss=ss: e.activation(jk[:], xt[:], AF.Square, accum_out=ss[:, 0:1]),
              reads=[xb], writes=[jb, ssb])
        ph.op("act", lambda e, ss=ss: e.activation(ss[:, 1:2], ss[:, 0:1], AF.Sqrt, bias=res["eps"][:, 0:1], scale=1.0 / D),
              reads=[ssb, res["epsb"]], writes=[ssb])
        ph.op("dve", lambda e, ss=ss: e.reciprocal(ss[:, 2:3], ss[:, 1:2]), reads=[ssb], writes=[ssb])
        ph.op("dve", lambda e, xs=xs, xt=xt, ss=ss: e.tensor_scalar(xs[:], xt[:], ss[:, 2:3], None, ALU.mult),
              reads=[xb, ssb], writes=[xsb])
        for c0 in range(0, DC, 8):
            nch = min(8, DC - c0)
            tp, tpb = res["tp"].next()
            for k in range(nch):
                c = c0 + k
                ph.op("pe", lambda e, tp=tp, xs=xs, c=c, k=k: e.transpose(tp[:, k * 128:(k + 1) * 128], xs[:, c * 128:(c + 1) * 128], ident[:]),
                      reads=[xsb, identb], writes=[tpb])
            ph.op("dve", lambda e, tp=tp, c0=c0, nch=nch, t=t: e.tensor_tensor(
                hT[:, c0:c0 + nch, t * 128:(t + 1) * 128],
                tp[:, 0:nch * 128].rearrange("p (c n) -> p c n", n=128),
                gT[:, c0:c0 + nch].unsqueeze(2).to_broadcast([128, nch, 128]), ALU.mult),
                reads=[tpb, gTb], writes=[hTb])


def load_w_bf16(ph, res, w_cols, DC, ncols, wbf, wbfb, cast_rr):
    wv = w_cols.rearrange("(c p) n -> p c n", p=128)
    step = res["wst_chunks"]
    for c0 in range(0, DC, step):
        n = min(step, DC - c0)
        stg, stgb = res["wst"].next()
        ph.dma(stg[:, 0:n, 0:ncols], wv[:, c0:c0 + n, :], writes=[stgb])
        eng = cast_rr.next()
        if eng == "act":
            ph.op("act", lambda e, stg=stg, c0=c0, n=n: e.copy(wbf[:, c0:c0 + n, 0:ncols], stg[:, 0:n, 0:ncols]),
                  reads=[stgb], writes=[wbfb])
        else:
            ph.op(eng, lambda e, stg=stg, c0=c0, n=n: e.tensor_copy(wbf[:, c0:c0 + n, 0:ncols], stg[:, 0:n, 0:ncols]),
                  reads=[stgb], writes=[wbfb])


def phase_A(nc, cfg, io, scr):
    D, DC, T, TQ = cfg.D, cfg.DC, cfg.T, cfg.TQ
    W = cfg.HM * 128
    scale = 128 ** -0.5
    for (src, nrows, tag) in ((io["xb"], T, "kv"), (io["xq"], TQ, "q")):
        GT = min(1024, nrows)
        NS = GT // 512
        for g in range(nrows // GT):
            with ExitStack() as st:
                P = Pools(nc, st)
                ph = Phase(nc, "A%s%d" % (tag, g))
                ident, identb = load_const(ph, P, io["ident_bf"], [128, 128], BF16, "ident")
                ones, onesb = load_const(ph, P, io["ones_bf"], [128, 128], BF16, "ones")
                gT, gTb = load_vecT(nc, ph, P, io["norm_mix_g"], DC, "gT")
                gq, gqb = load_vecT(nc, ph, P, io["moba_q_norm_g"], 1, "gq")
                gk, gkb = load_vecT(nc, ph, P, io["moba_k_norm_g"], 1, "gk")
                eps, epsb = P.sb([128, 1], F32, "eps")
                ph.op("pool", lambda e: e.memset(eps[:], RMS_EPS), writes=[epsb])
                hT, hTb = P.sb([128, DC, GT], BF16, "hT")
                res = {
                    "xt": Ring([P.sb([128, D], F32, "xt") for _ in range(2)]),
                    "ss": Ring([P.sb([128, 4], F32, "ss") for _ in range(2)]),
                    "xs": Ring([P.sb([128, D], BF16, "xs") for _ in range(2)]),
                    "tp": Ring([P.ps([128, 1024], BF16, "tp") for _ in range(2)]),
                    "eps": eps, "epsb": epsb,
                    "wst_chunks": 16,
                    "wst": Ring([P.sb([128, 16, 256], F32, "wst") for _ in range(2)]),
                }
                for sub in range(NS):
                    build_hT(nc, cfg, ph, P, st, src[g * GT + sub * 512:g * GT + (sub + 1) * 512, :], 512, gT, gTb, ident, identb,
                             hT[:, :, sub * 512:(sub + 1) * 512], hTb, res)
                wring = Ring([P.sb([128, DC, 256], BF16, "wbf") for _ in range(2)])
                mm = Ring([P.ps([128, 512], F32, "mm") for _ in range(3)])
                sq_ps = Ring([P.ps([128, 512], F32, "sqp") for _ in range(2)])
                sqt = Ring([P.sb([128, 512], BF16, "sqt") for _ in range(2)])
                srt = Ring([P.sb([128, 512], F32, "srt") for _ in range(2)])
                obf = Ring([P.sb([128, 512], BF16, "obf") for _ in range(4)])
                vbf = Ring([P.sb([128, 4, 256], BF16, "vbf") for _ in range(2)])
                cast_rr = Ring(["act", "dve"])
                ev_rr = Ring(["act", "dve"])
                if tag == "kv":
                    jobs = []
                    for h0 in range(0, cfg.HM, 2):
                        jobs.append(("KM", W + h0 * 128, h0))
                        jobs.append(("VM", 2 * W + h0 * 128, h0))
                    for h0 in range(0, cfg.HS, 2):
                        jobs.append(("KS", 4 * W + h0 * 128, h0))
                        jobs.append(("VS", 5 * W + h0 * 128, h0))
                else:
                    jobs = []
                    for h0 in range(0, cfg.HM, 2):
                        jobs.append(("QM", 0 + h0 * 128, h0))
                    for h0 in range(0, cfg.HS, 2):
                        jobs.append(("QS", 3 * W + h0 * 128, h0))
                for (kind, col0, h0) in jobs:
                    wbf, wbfb = wring.next()
                    load_w_bf16(ph, res, io["w_in"][:, col0:col0 + 256], DC, 256, wbf, wbfb, cast_rr)
                    for sub in range(NS):
                      tsl = slice(g * GT + sub * 512, g * GT + (sub + 1) * 512)
                      hTs = hT[:, :, sub * 512:(sub + 1) * 512]
                      if kind in ("VM", "VS"):
                          vt, vtb = vbf.next()
                          for t in range(4):
                              ps, psb = mm.next()
                              for c in range(DC):
                                  ph.op("pe", lambda e, ps=ps, c=c, t=t, wbf=wbf, hTs=hTs: e.matmul(
                                      ps[:, 0:256], lhsT=hTs[:, c, t * 128:(t + 1) * 128], rhs=wbf[:, c, 0:256],
                                      start=(c == 0), stop=(c == DC - 1)), reads=[hTb, wbfb], writes=[psb])
                              eng = ev_rr.next()
                              if eng == "act":
                                  ph.op("act", lambda e, ps=ps, vt=vt, t=t: e.copy(vt[:, t, :], ps[:, 0:256]), reads=[psb], writes=[vtb])
                              else:
                                  ph.op("dve", lambda e, ps=ps, vt=vt, t=t: e.tensor_copy(vt[:, t, :], ps[:, 0:256]), reads=[psb], writes=[vtb])
                          dst = scr["vm"] if kind == "VM" else scr["vs"]
                          for hh in range(2):
                              ph.dma(dst[h0 + hh, tsl, :].rearrange("(t p) d -> p t d", p=128),
                                     vt[:, :, hh * 128:(hh + 1) * 128], reads=[vtb], q="pool")
                      else:
                          for ct in range(2):
                              h = h0 + ct
                              ps, psb = mm.next()
                              for c in range(DC):
                                  ph.op("pe", lambda e, ps=ps, c=c, ct=ct, wbf=wbf, hTs=hTs: e.matmul(
                                      ps[:], lhsT=wbf[:, c, ct * 128:(ct + 1) * 128], rhs=hTs[:, c, :],
                                      start=(c == 0), stop=(c == DC - 1)), reads=[hTb, wbfb], writes=[psb])
                              if kind in ("KM", "QM"):
                                  sq, sqb = sqt.next()
                                  ph.op("act", lambda e, sq=sq, ps=ps: e.activation(sq[:], ps[:], AF.Square), reads=[psb], writes=[sqb])
                                  sp_, spb = sq_ps.next()
                                  ph.op("pe", lambda e, sp_=sp_, sq=sq: e.matmul(sp_[:], lhsT=ones[:], rhs=sq[:], start=True, stop=True),
                                        reads=[sqb, onesb], writes=[spb])
                                  sr, srb = srt.next()
                                  ph.op("act", lambda e, sr=sr, sp_=sp_: e.activation(sr[:], sp_[:], AF.Sqrt, bias=eps[:, 0:1], scale=1.0 / 128),
                                        reads=[spb, epsb], writes=[srb])
                                  ph.op("dve", lambda e, sr=sr: e.reciprocal(sr[:], sr[:]), reads=[srb], writes=[srb])
                                  o, ob = obf.next()
                                  gg, ggb = (gk, gkb) if kind == "KM" else (gq, gqb)
                                  ph.op("dve", lambda e, o=o, ps=ps, sr=sr, gg=gg: e.scalar_tensor_tensor(
                                      o[:], ps[:], gg[:, 0:1], sr[:], ALU.mult, ALU.mult), reads=[psb, srb, ggb], writes=[ob])
                                  dst = scr["kTm"] if kind == "KM" else scr["qTm"]
                                  ph.dma(dst[h, :, tsl], o[:], reads=[ob], q="pool")
                              elif kind == "KS":
                                  o, ob = obf.next()
                                  ph.op("act", lambda e, o=o, ps=ps: e.copy(o[:], ps[:]), reads=[psb], writes=[ob])
                                  ph.dma(scr["kTs"][h, :, tsl], o[:], reads=[ob], q="pool")
                                  o2, ob2 = obf.next()
                                  ph.op("dve", lambda e, o2=o2, ps=ps: e.tensor_scalar(o2[:], ps[:], -scale, None, ALU.mult), reads=[psb], writes=[ob2])
                                  ph.dma(scr["nkTs"][h, :, tsl], o2[:], reads=[ob2], q="pool")
                              else:
                                  o, ob = obf.next()
                                  ph.op("act", lambda e, o=o, ps=ps: e.copy(o[:], ps[:]), reads=[psb], writes=[ob])
                                  ph.dma(scr["qTs"][h, :, tsl], o[:], reads=[ob], q="pool")
                ph.emit()


def declare_io(nc, cfg, debug=()):
    D, T, TQ = cfg.D, cfg.T, cfg.TQ
    W = cfg.HM * 128

    shapes = {}

    def inp(name, shape, dt=F32):
        shapes[name] = (list(shape), dt)
        return nc.dram_tensor(name, list(shape), dt, kind="ExternalInput").ap()

    io = {
        "xb": inp("xb", [T, D]), "xq": inp("xq", [TQ, D]), "memb": inp("memb", [cfg.NMEM, D]),
        "norm_mix_g": inp("norm_mix_g", [D]), "w_in": inp("w_in", [D, 3 * cfg.MIX]),
        "moba_q_norm_g": inp("moba_q_norm_g", [128]), "moba_k_norm_g": inp("moba_k_norm_g", [128]),
        "moba_out_norm_g": inp("moba_out_norm_g", [W]), "sb_out_norm_g": inp("sb_out_norm_g", [W]),
        "w_out": inp("w_out", [cfg.MIX, D]), "norm_xattn_g": inp("norm_xattn_g", [D]),
        "norm_mem_g": inp("norm_mem_g", [D]), "w_xq": inp("w_xq", [D, cfg.XW]),
        "w_xkv": inp("w_xkv", [D, 2 * cfg.XW]), "xattn_q_norm_g": inp("xattn_q_norm_g", [128]),
        "xattn_k_norm_g": inp("xattn_k_norm_g", [128]), "w_xo": inp("w_xo", [cfg.XW, D]),
        "norm_ffn_g": inp("norm_ffn_g", [D]), "w_peer_q": inp("w_peer_q", [D, cfg.PW]),
        "peer_sub_keys": inp("peer_sub_keys", [16, 128, 128]),
        "peer_u": inp("peer_u", [cfg.NE, D]), "peer_v": inp("peer_v", [cfg.NE, D]),
        "ident_bf": inp("ident_bf", [128, 128], BF16), "ones_bf": inp("ones_bf", [128, 128], BF16),
        "ident_f32": inp("ident_f32", [128, 128]),
        "tri_bf": inp("tri_bf", [128, 128], BF16),
        "moba_bias": inp("moba_bias", [cfg.HM, 128, cfg.NOWN * cfg.NB * 2]),
        "moba_alt": inp("moba_alt", [cfg.HM, 3, 256], BF16),
        "moba_sel": inp("moba_sel", [128, cfg.NB * 128], BF16),
        "gate_mask": inp("gate_mask", [128, cfg.NOWN * 2 * 16]),
        "cmask_moba": inp("cmask_moba", [128, 8 * 256], BF16),
        "cmask_sb": inp("cmask_sb", [128, 8 * 256], BF16),
    }
    io["y"] = nc.dram_tensor("y", [TQ, D], F32, kind="ExternalOutput").ap()
    io["_shapes"] = shapes

    def scratch(name, shape, dt):
        if name in debug:
            return nc.dram_tensor(name, list(shape), dt, kind="ExternalOutput").ap()
        return nc.dram_tensor(name, list(shape), dt).ap()

    scr = {
        "kTm": scratch("kTm", [cfg.HM, 128, T], BF16), "kTs": scratch("kTs", [cfg.HS, 128, T], BF16),
        "nkTs": scratch("nkTs", [cfg.HS, 128, T], BF16),
        "vm": scratch("vm", [cfg.HM, T, 128], BF16), "vs": scratch("vs", [cfg.HS, T, 128], BF16),
        "qTm": scratch("qTm", [cfg.HM, 128, TQ], BF16), "qTs": scratch("qTs", [cfg.HS, 128, TQ], BF16),
        "OT": scratch("OT", [cfg.HM + cfg.HS, 128, TQ], BF16),
        "x1": scratch("x1", [TQ, D], F32), "x2": scratch("x2", [TQ, D], F32),
        "kTx": scratch("kTx", [4, 128, cfg.NMEM], BF16), "vx": scratch("vx", [cfg.NMEM, 512], BF16),
        "uT": scratch("uT", [cfg.NE // 128, 128, cfg.DC, 128], BF16), "vbf": scratch("vbf", [cfg.NE, D], BF16),
        "hTf": scratch("hTf", [TQ // 256, 128, cfg.DC, 256], BF16),
        "sall": scratch("sall", [TQ // 128, 128, 8, 128], F32), "s1p": scratch("s1p", [TQ // 128, 128, 8, 128], F32),
        "thr": scratch("thr", [TQ // 128, 128, 8], F32),
    }
    return io, scr


def alibi_slopes_np(n):
    return (2.0 ** (-8.0 * np.arange(1, n + 1) / n)).astype(np.float64)


def split3_bf16(a):
    a = np.asarray(a, np.float64)
    hi = a.astype(ml_dtypes.bfloat16)
    r = a - hi.astype(np.float64)
    mid = r.astype(ml_dtypes.bfloat16)
    r2 = r - mid.astype(np.float64)
    lo = r2.astype(ml_dtypes.bfloat16)
    return hi, mid, lo


def host_consts(cfg, p):
    bf = ml_dtypes.bfloat16
    scale = 128 ** -0.5
    NB, NOWN, HM = cfg.NB, cfg.NOWN, cfg.HM
    own = own_blocks(cfg, p)
    c = {}
    c["ident_bf"] = np.eye(128, dtype=np.float32).astype(bf)
    c["ident_f32"] = np.eye(128, dtype=np.float32)
    c["ones_bf"] = np.ones((128, 128), np.float32).astype(bf)
    jj = np.arange(128)
    c["tri_bf"] = (jj[:, None] >= jj[None, :]).astype(np.float32).astype(bf)
    slopes = alibi_slopes_np(HM)
    mb = np.zeros((HM, 128, NOWN, NB, 2), np.float64)
    for i in range(NOWN):
        for n in range(NB):
            for kt in range(2):
                jabs = n * 256 + kt * 128 + jj
                val = jabs - own[i] * 256 - 128
                if n > own[i]:
                    mb[:, :, i, n, kt] = -BIG
                else:
                    mb[:, :, i, n, kt] = slopes[:, None] * val[None, :]
    c["moba_bias"] = mb.reshape(HM, 128, NOWN * NB * 2).astype(np.float32)
    tt = np.arange(256)
    alt = (-slopes[:, None] * (tt[None, :] - 128)) / scale
    hi, mid, lo = split3_bf16(alt)
    c["moba_alt"] = np.stack([hi, mid, lo], axis=1)
    sel = np.zeros((128, NB, 128), np.float32)
    for n in range(NB):
        sel[n, n, :] = 1.0
    sel[16:19, :, :] = 1.0
    c["moba_sel"] = sel.reshape(128, NB * 128).astype(bf)
    gm = np.zeros((128, NOWN, 2, 16), np.float32)
    for i in range(NOWN):
        for n in range(16):
            gm[:, i, 0, n] = 0.0 if n < own[i] else -1e30
            gm[:, i, 1, n] = BIG if n < own[i] else 0.0
    c["gate_mask"] = gm[:, :, :, :].reshape(128, NOWN * 2 * 16)
    def cm(strict):
        m = np.zeros((2, 2, 2, 128, 256), np.float32)
        for case in range(2):
            for slot in range(2):
                for kt in range(2):
                    jabs = slot * 256 + kt * 128 + jj
                    tabs = case * 256 + tt
                    if strict:
                        ok = jabs[:, None] < tabs[None, :]
                    else:
                        ok = jabs[:, None] <= tabs[None, :]
                    m[case, slot, kt] = ok
        return m
    def percore(m):
        out = np.zeros((128, 2, 2, 2, 256), np.float32)
        for ipar in range(2):
            case = p if ipar == 0 else 1 - p
            for slot in range(2):
                for kt in range(2):
                    out[:, ipar, slot, kt, :] = m[case, slot, kt]
        return out.reshape(128, 8 * 256)
    c["cmask_moba"] = ((percore(cm(False)) - 1.0) * BIG).astype(bf)
    c["cmask_sb"] = percore(cm(True)).astype(bf)
    return c


def f0_res(ph, P, io):
    R = {}
    R["ident"], R["identb"] = load_const(ph, P, io["ident_bf"], [128, 128], BF16, "f0ident")
    return R


def f0_alloc(P, cfg, R):
    D, DC = cfg.D, cfg.DC
    R["ut"] = Ring([P.sb([128, D], F32, "f0ut") for _ in range(2)])
    R["ub"] = Ring([P.sb([128, D], BF16, "f0ub") for _ in range(2)])
    R["vt"] = Ring([P.sb([128, D], F32, "f0vt") for _ in range(2)])
    R["vb"] = Ring([P.sb([128, D], BF16, "f0vb") for _ in range(2)])
    R["uTs"] = Ring([P.sb([128, DC, 128], BF16, "f0uTs") for _ in range(2)])
    R["tp"] = Ring([P.ps([128, 1024], BF16, "f0tp") for _ in range(1)])
    R["st"] = {}


def f0_stageA(ph, cfg, io, scr, R, i):
    rows = slice(i * 128, (i + 1) * 128)
    u_, u_b = R["ut"].next()
    ph.dma(u_[:], io["peer_u"][rows, :], writes=[u_b])
    ubf, ubfb = R["ub"].next()
    ph.op("act", lambda e: e.copy(ubf[:], u_[:]), reads=[u_b], writes=[ubfb])
    R["st"][i] = (ubf, ubfb)
    v_, v_b = R["vt"].next()
    ph.dma(v_[:], io["peer_v"][rows, :], writes=[v_b])
    vbf, vbfb = R["vb"].next()
    ph.op("dve", lambda e: e.tensor_copy(vbf[:], v_[:]), reads=[v_b], writes=[vbfb])
    ph.dma(scr["vbf"][rows, :], vbf[:], reads=[vbfb], q="pool")


def f0_stageB(ph, cfg, io, scr, R, i):
    DC = cfg.DC
    ubf, ubfb = R["st"].pop(i)
    ident, identb = R["ident"], R["identb"]
    uo, uob = R["uTs"].next()
    for c0 in range(0, DC, 8):
        nch = min(8, DC - c0)
        t_, t_b = R["tp"].next()
        for k in range(nch):
            c = c0 + k
            ph.op("pe", lambda e, t_=t_, c=c, k=k: e.transpose(t_[:, k * 128:(k + 1) * 128], ubf[:, c * 128:(c + 1) * 128], ident[:]),
                  reads=[ubfb, identb], writes=[t_b])
        ph.op("dve", lambda e, t_=t_, c0=c0, nch=nch: e.tensor_copy(uo[:, c0:c0 + nch, :], t_[:, 0:nch * 128].rearrange("p (c n) -> p c n", n=128)),
              reads=[t_b], writes=[uob])
    ph.dma(scr["uT"][i], uo[:], reads=[uob], q="pool")


class F0Sched:
    def __init__(s, cfg):
        s.cfg = cfg
        s.next_chunk = 0
        s.nheads = cfg.HM
        s.nch = cfg.NE // 128
        s.per_head = -(-s.nch // s.nheads)

    def begin_head(s):
        n = min(s.per_head, s.nch - s.next_chunk)
        s.cur = list(range(s.next_chunk, s.next_chunk + n))
        s.next_chunk += n
        s.a_done = 0
        s.b_done = 0

    def tick(s, ph, io, scr, R, frac):
        n = len(s.cur)
        if n == 0:
            return
        ta = min(n, int(frac * (n + 1)) + 1)
        tb = n if frac >= 1.0 else max(0, ta - 1)
        while s.a_done < ta or s.b_done < tb:
            if s.a_done < ta and s.a_done - s.b_done < 2:
                f0_stageA(ph, s.cfg, io, scr, R, s.cur[s.a_done])
                s.a_done += 1
            elif s.b_done < s.a_done:
                f0_stageB(ph, s.cfg, io, scr, R, s.cur[s.b_done])
                s.b_done += 1
            else:
                break

    def finish(s, ph, io, scr, R):
        s.tick(ph, io, scr, R, 2.0)


def phase_B(nc, cfg, io, scr, f0=None):
    T, TQ, NB, NOWN = cfg.T, cfg.TQ, cfg.NB, cfg.NOWN
    KT = T // 128
    scale = 128 ** -0.5
    for h in range(cfg.HM):
        with ExitStack() as st:
            P = Pools(nc, st)
            ph = Phase(nc, "B%d" % h)
            if f0 is not None:
                R0 = f0_res(ph, P, io)
                f0_alloc(P, cfg, R0)
                f0.begin_head()
            identf, identfb = load_const(ph, P, io["ident_f32"], [128, 128], F32, "identf")
            ones, onesb = load_const(ph, P, io["ones_bf"], [128, 128], BF16, "ones")
            sel, selb = load_const(ph, P, io["moba_sel"], [128, NB * 128], BF16, "sel")
            gmask, gmaskb = load_const(ph, P, io["gate_mask"], [128, NOWN * 2 * 16], F32, "gmask")
            cmask, cmaskb = load_const(ph, P, io["cmask_moba"], [128, 8 * 256], BF16, "cmask")
            bias, biasb = load_const(ph, P, io["moba_bias"][h], [128, NOWN * NB * 2], F32, "bias")
            kT, kTb = load_const(ph, P, scr["kTm"][h], [128, T], BF16, "kT")
            qT, qTb = load_const(ph, P, scr["qTm"][h], [128, TQ], BF16, "qT")
            vv, vvb = P.sb([128, KT, 128], BF16, "vv")
            ph.dma(vv[:], scr["vm"][h].rearrange("(k p) d -> p k d", p=128), writes=[vvb])
            mrhs_all, mrhsb = P.sb([128, NOWN, 256], BF16, "mrhs")
            ph.op("pool", lambda e: e.memset(mrhs_all[:], 0.0), writes=[mrhsb])
            for i in range(NOWN):
                ph.dma(mrhs_all[16:19, i, :], io["moba_alt"][h], writes=[mrhsb])
            kmf, kmfb = P.sb([128, 16], F32, "kmf")
            km, kmb = P.sb([128, 16], BF16, "km")
            ph.op("dve", lambda e: e.tensor_reduce(kmf[:, 0:NB], kT[:].rearrange("p (n j) -> p n j", j=256), AX.X, ALU.add),
                  reads=[kTb], writes=[kmfb])
            ph.op("dve", lambda e: e.tensor_scalar(km[:, 0:NB], kmf[:, 0:NB], 1.0 / 256, None, ALU.mult), reads=[kmfb], writes=[kmb])
            gmr = Ring([P.sb([128, 16], F32, "gm") for _ in range(2)])
            for (gm_, gmb_) in gmr.items:
                ph.op("pool", lambda e, gm_=gm_: e.memset(gm_[:], -3.0e38), writes=[gmb_])
            m8r = Ring([P.sb([128, 8], F32, "m8") for _ in range(2)])
            svr = Ring([P.sb([128, 16], F32, "selv") for _ in range(2)])
            ngr = Ring([P.sb([128, 16], F32, "negm") for _ in range(2)])
            gpr = Ring([P.ps([128, 512], F32, "gps") for _ in range(1)])
            tpr = gpr
            sps = Ring([P.ps([128, 512], F32, "sps") for _ in range(2)])
            ops_ = Ring([P.ps([128, 512], F32, "ops") for _ in range(2)])
            dps = Ring([P.ps([128, 512], F32, "dps") for _ in range(2)])
            pbf = Ring([P.sb([128, 256], BF16, "pbf") for _ in range(4)])
            tmp = Ring([P.sb([128, 256], F32, "tmp") for _ in range(2)])
            rd, rdb = P.sb([128, 256], F32, "rden")
            obf = Ring([P.sb([128, 256], BF16, "obf") for _ in range(2)])

            def gate(i, qt):
                gps, gpsb = gpr.next()
                tps, tpsb = tpr.next()
                gm, gmb = gmr.next()
                m8, m8b = m8r.next()
                sv_, svb = svr.next()
                ng, ngb = ngr.next()
                ph.op("pe", lambda e: e.matmul(gps[:, 0:NB], lhsT=qT[:, i * 256 + qt * 128:i * 256 + (qt + 1) * 128],
                                               rhs=km[:, 0:NB], start=True, stop=True), reads=[qTb, kmb], writes=[gpsb])
                g0 = (i * 2 + 0) * 16
                g1 = (i * 2 + 1) * 16
                ph.op("dve", lambda e: e.tensor_tensor(gm[:, 0:NB], gps[:, 0:NB], gmask[:, g0:g0 + NB], ALU.add), reads=[gpsb, gmaskb], writes=[gmb])
                ph.op("dve", lambda e: e.max(m8[:], gm[:]), reads=[gmb], writes=[m8b])
                ph.op("dve", lambda e: e.scalar_tensor_tensor(sv_[:], gm[:], m8[:, 2:3], gmask[:, g1:g1 + 16], ALU.is_ge, ALU.mult),
                      reads=[gmb, m8b, gmaskb], writes=[svb])
                ph.op("dve", lambda e: e.tensor_tensor(ng[:], sv_[:], gmask[:, g1:g1 + 16], ALU.subtract), reads=[svb, gmaskb], writes=[ngb])
                ph.op("pe", lambda e: e.transpose(tps[0:16, 128:256], ng[:], identf[:]), reads=[ngb, identfb], writes=[tpsb])
                ph.op("act", lambda e: e.copy(mrhs_all[0:16, i, qt * 128:(qt + 1) * 128], tps[0:16, 128:256]), reads=[tpsb], writes=[mrhsb])

            for i in range(NOWN):
                for qt in range(2):
                    gate(i, qt)
            tiles = []
            for i in range(NOWN):
                nkt = (2 * i + 2) * 2
                for kti in range(nkt):
                    tiles.append({"i": i, "kti": kti, "first": kti == 0, "last": kti == nkt - 1})
            blk = {}

            def s1(tl):
                i, kti = tl["i"], tl["kti"]
                qs = slice(i * 256, (i + 1) * 256)
                n, kt = kti // 2, kti % 2
                S, Sb = sps.next()
                ph.op("pe", lambda e: e.matmul(S[:, 0:256], lhsT=kT[:, kti * 128:(kti + 1) * 128], rhs=qT[:, qs], start=True, stop=False),
                      reads=[kTb, qTb], writes=[Sb])
                ph.op("pe", lambda e: e.matmul(S[:, 0:256], lhsT=sel[:, n * 128:(n + 1) * 128], rhs=mrhs_all[:, i, :], start=False, stop=True),
                      reads=[selb, mrhsb], writes=[Sb])
                bcol = (i * NB + n) * 2 + kt
                Pt, Ptb = pbf.next()
                if n >= 2 * i:
                    cidx = ((i % 2) * 2 + (n - 2 * i)) * 2 + kt
                    tm, tmb = tmp.next()
                    ph.op("dve", lambda e: e.tensor_tensor(tm[:], S[:, 0:256], cmask[:, cidx * 256:(cidx + 1) * 256], ALU.add), reads=[Sb, cmaskb], writes=[tmb])
                    ph.op("act", lambda e: e.activation(Pt[:], tm[:], AF.Exp, bias=bias[:, bcol:bcol + 1], scale=scale), reads=[tmb, biasb], writes=[Ptb])
                else:
                    ph.op("act", lambda e: e.activation(Pt[:], S[:, 0:256], AF.Exp, bias=bias[:, bcol:bcol + 1], scale=scale), reads=[Sb, biasb], writes=[Ptb])
                tl["P"], tl["Pb"] = Pt, Ptb

            def s2(tl):
                i, kti = tl["i"], tl["kti"]
                qs = slice(i * 256, (i + 1) * 256)
                if tl["first"]:
                    blk[i] = ops_.next() + dps.next()
                O, Ob, Dn, Dnb = blk[i]
                Pt, Ptb = tl["P"], tl["Pb"]
                ph.op("pe", lambda e: e.matmul(O[:, 0:256], lhsT=vv[:, kti, :], rhs=Pt[:], start=tl["first"], stop=tl["last"]), reads=[vvb, Ptb], writes=[Ob])
                ph.op("pe", lambda e: e.matmul(Dn[:, 0:256], lhsT=ones[:], rhs=Pt[:], start=tl["first"], stop=tl["last"]), reads=[onesb, Ptb], writes=[Dnb])
                if tl["last"]:
                    ph.op("dve", lambda e: e.reciprocal(rd[:], Dn[:, 0:256]), reads=[Dnb], writes=[rdb])
                    o, ob = obf.next()
                    ph.op("dve", lambda e: e.tensor_tensor(o[:], O[:, 0:256], rd[:], ALU.mult), reads=[Ob, rdb], writes=[ob])
                    ph.dma(scr["OT"][h, :, qs], o[:], reads=[ob], q="pool")

            NT_ = len(tiles)
            for k in range(NT_ + 2):
                if f0 is not None:
                    f0.tick(ph, io, scr, R0, k / float(NT_ + 2))
                if k < NT_:
                    s1(tiles[k])
                if 0 <= k - 2 < NT_:
                    s2(tiles[k - 2])
            if f0 is not None:
                f0.finish(ph, io, scr, R0)
            ph.emit()


def phase_C(nc, cfg, io, scr, f0=None):
    T, TQ, NB, NOWN = cfg.T, cfg.TQ, cfg.NB, cfg.NOWN
    KT = T // 128
    scale = 128 ** -0.5
    for h in range(cfg.HS):
        with ExitStack() as st:
            P = Pools(nc, st)
            ph = Phase(nc, "C%d" % h)
            if f0 is not None:
                R0 = f0_res(ph, P, io)
                f0_alloc(P, cfg, R0)
                f0.begin_head()
            ones, onesb = load_const(ph, P, io["ones_bf"], [128, 128], BF16, "ones")
            tri, trib = load_const(ph, P, io["tri_bf"], [128, 128], BF16, "tri")
            cmask, cmaskb = load_const(ph, P, io["cmask_sb"], [128, 8 * 256], BF16, "cmask")
            kT, kTb = load_const(ph, P, scr["kTs"][h], [128, T], BF16, "kT")
            nkT, nkTb = load_const(ph, P, scr["nkTs"][h], [128, T], BF16, "nkT")
            qT, qTb = load_const(ph, P, scr["qTs"][h], [128, TQ], BF16, "qT")
            vv, vvb = P.sb([128, KT, 128], BF16, "vv")
            ph.dma(vv[:], scr["vs"][h].rearrange("(k p) d -> p k d", p=128), writes=[vvb])
            one1, one1b = P.sb([128, 1], F32, "one1")
            ph.op("pool", lambda e: e.memset(one1[:], 1.0), writes=[one1b])
            zps = Ring([P.ps([128, 512], F32, "zps") for _ in range(3)])
            cps = Ring([P.ps([128, 512], F32, "cps") for _ in range(2)])
            ops_ = Ring([P.ps([128, 512], F32, "ops") for _ in range(2)])
            Ef = Ring([P.sb([128, 512], F32, "Ef") for _ in range(3)])
            Lb = Ring([P.sb([128, 512], BF16, "Lb") for _ in range(5)])
            ab = Ring([P.sb([128, 512], BF16, "ab") for _ in range(4)])
            Ls = Ring([P.sb([128, 256], BF16, "Lsum") for _ in range(2)])
            obf = Ring([P.sb([128, 256], BF16, "obf") for _ in range(2)])
            tiles = []
            for i in range(NOWN):
                npair = 2 * i + 2
                for m in range(npair - 1, -1, -1):
                    tiles.append({"i": i, "m": m, "first": m == npair - 1, "last": m == 0})
            blk = {}

            def s1(tl):
                i, m = tl["i"], tl["m"]
                qs = slice(i * 256, (i + 1) * 256)
                if tl["first"]:
                    O, Ob = ops_.next()
                    Lsum, Lsumb = Ls.next()
                    ph.op("dve", lambda e: e.memset(Lsum[:], 0.0), writes=[Lsumb])
                    blk[i] = (O, Ob, Lsum, Lsumb)
                Z, Zb = zps.next()
                for hf in range(2):
                    kti = 2 * m + hf
                    ph.op("pe", lambda e, hf=hf, kti=kti: e.matmul(Z[:, hf * 256:(hf + 1) * 256], lhsT=kT[:, kti * 128:(kti + 1) * 128], rhs=qT[:, qs], start=True, stop=True),
                          reads=[kTb, qTb], writes=[Zb])
                E, Eb = Ef.next()
                ph.op("act", lambda e: e.activation(E[:], Z[:], AF.Exp, scale=scale), reads=[Zb], writes=[Eb])
                L, Lbb = Lb.next()
                ph.op("act", lambda e: e.activation(L[:], E[:], AF.Ln, bias=one1[:, 0:1], scale=1.0), reads=[Eb, one1b], writes=[Lbb])
                c0 = None
                if m >= 2 * i:
                    c0 = ((i % 2) * 2 + (m - 2 * i)) * 2 * 256
                    ph.op("dve", lambda e: e.tensor_tensor(L[:], L[:], cmask[:, c0:c0 + 512], ALU.mult), reads=[cmaskb], writes=[Lbb])
                tl["L"], tl["Lbb"], tl["c0"] = L, Lbb, c0

            def s2a(tl):
                i, m = tl["i"], tl["m"]
                qs = slice(i * 256, (i + 1) * 256)
                O, Ob, Lsum, Lsumb = blk[i]
                L, Lbb, c0 = tl["L"], tl["Lbb"], tl["c0"]
                C, Cb = cps.next()
                for hf in (1, 0):
                    kti = 2 * m + hf
                    cs = slice(hf * 256, (hf + 1) * 256)
                    ph.op("pe", lambda e, kti=kti, cs=cs: e.matmul(C[:, cs], lhsT=nkT[:, kti * 128:(kti + 1) * 128], rhs=qT[:, qs], start=True, stop=False),
                          reads=[nkTb, qTb], writes=[Cb])
                    ph.op("pe", lambda e, cs=cs: e.matmul(C[:, cs], lhsT=tri[:], rhs=L[:, cs], start=False, stop=False), reads=[trib, Lbb], writes=[Cb])
                    ph.op("pe", lambda e, cs=cs, hf=hf: e.matmul(C[:, cs], lhsT=ones[:], rhs=Lsum[:], start=False, stop=(hf == 1)), reads=[onesb, Lsumb], writes=[Cb])
                    if hf == 0:
                        ph.op("pe", lambda e, cs=cs: e.matmul(C[:, cs], lhsT=ones[:], rhs=L[:, 256:512], start=False, stop=True), reads=[onesb, Lbb], writes=[Cb])
                if not tl["last"]:
                    ph.op("dve", lambda e: e.tensor_tensor(Lsum[:], Lsum[:], L[:, 0:256], ALU.add), reads=[Lbb], writes=[Lsumb])
                    ph.op("dve", lambda e: e.tensor_tensor(Lsum[:], Lsum[:], L[:, 256:512], ALU.add), reads=[Lbb], writes=[Lsumb])
                a_, abb = ab.next()
                ph.op("act", lambda e: e.activation(a_[:], C[:], AF.Exp, scale=-1.0), reads=[Cb], writes=[abb])
                if c0 is not None:
                    ph.op("dve", lambda e: e.tensor_tensor(a_[:], a_[:], cmask[:, c0:c0 + 512], ALU.mult), reads=[cmaskb], writes=[abb])
                tl["a"], tl["abb"] = a_, abb

            def s2b(tl):
                i, m = tl["i"], tl["m"]
                qs = slice(i * 256, (i + 1) * 256)
                O, Ob, Lsum, Lsumb = blk[i]
                a_, abb = tl["a"], tl["abb"]
                for hf in (1, 0):
                    kti = 2 * m + hf
                    ph.op("pe", lambda e, kti=kti, hf=hf: e.matmul(O[:, 0:256], lhsT=vv[:, kti, :], rhs=a_[:, hf * 256:(hf + 1) * 256],
                                                                  start=(tl["first"] and hf == 1), stop=(tl["last"] and hf == 0)), reads=[vvb, abb], writes=[Ob])
                if tl["last"]:
                    o, ob = obf.next()
                    ph.op("dve", lambda e: e.tensor_copy(o[:], O[:, 0:256]), reads=[Ob], writes=[ob])
                    ph.dma(scr["OT"][cfg.HM + h, :, qs], o[:], reads=[ob], q="pool")

            NT_ = len(tiles)
            for k in range(NT_ + 3):
                if f0 is not None:
                    f0.tick(ph, io, scr, R0, k / float(NT_ + 3))
                if k < NT_:
                    s1(tiles[k])
                if 0 <= k - 1 < NT_:
                    s2a(tiles[k - 1])
                if 0 <= k - 2 < NT_:
                    s2b(tiles[k - 2])
            if f0 is not None:
                f0.finish(ph, io, scr, R0)
            ph.emit()


def mk_norm_res(P):
    return {
        "sq_ps": Ring([P.ps([128, 512], F32, "sqp") for _ in range(2)]),
        "sqt": Ring([P.sb([128, 512], BF16, "sqt") for _ in range(2)]),
        "srt": Ring([P.sb([128, 512], F32, "srt") for _ in range(2)]),
    }


def qknorm_evac(ph, ps, psb, n, gg, ggb, ones, onesb, eps, epsb, nr, o, ob):
    sq, sqb = nr["sqt"].next()
    ph.op("act", lambda e: e.activation(sq[:, 0:n], ps[:, 0:n], AF.Square), reads=[psb], writes=[sqb])
    sp_, spb = nr["sq_ps"].next()
    ph.op("pe", lambda e: e.matmul(sp_[:, 0:n], lhsT=ones[:], rhs=sq[:, 0:n], start=True, stop=True), reads=[sqb, onesb], writes=[spb])
    sr, srb = nr["srt"].next()
    ph.op("act", lambda e: e.activation(sr[:, 0:n], sp_[:, 0:n], AF.Sqrt, bias=eps[:, 0:1], scale=1.0 / 128), reads=[spb, epsb], writes=[srb])
    ph.op("dve", lambda e: e.reciprocal(sr[:, 0:n], sr[:, 0:n]), reads=[srb], writes=[srb])
    ph.op("dve", lambda e: e.scalar_tensor_tensor(o, ps[:, 0:n], gg[:, 0:1], sr[:, 0:n], ALU.mult, ALU.mult), reads=[psb, srb, ggb], writes=[ob])


def hT_res(P, D, nxt=2, nxs=2):
    return {
        "xt": Ring([P.sb([128, D], F32, "xt") for _ in range(nxt)]),
        "ss": Ring([P.sb([128, 4], F32, "ss") for _ in range(2)]),
        "xs": Ring([P.sb([128, D], BF16, "xs") for _ in range(nxs)]),
        "tp": Ring([P.ps([128, 1024], BF16, "tp") for _ in range(2)]),
        "wst_chunks": 16,
        "wst": Ring([P.sb([128, 16, 256], F32, "wst") for _ in range(2)]),
    }


def std_consts(nc, ph, P, io):
    c = {}
    c["ident"], c["identb"] = load_const(ph, P, io["ident_bf"], [128, 128], BF16, "ident")
    c["ones"], c["onesb"] = load_const(ph, P, io["ones_bf"], [128, 128], BF16, "ones")
    c["eps"], c["epsb"] = P.sb([128, 1], F32, "eps")
    ph.op("pool", lambda e: e.memset(c["eps"][:], RMS_EPS), writes=[c["epsb"]])
    return c


def phase_D(nc, cfg, io, scr):
    D, DC, TQ = cfg.D, cfg.DC, cfg.TQ
    W = cfg.HM * 128
    NH = cfg.HM + cfg.HS
    GT = min(1024, TQ)
    for g in range(TQ // GT):
        with ExitStack() as st:
            P = Pools(nc, st)
            ph = Phase(nc, "D%d" % g)
            k = std_consts(nc, ph, P, io)
            ones, onesb, eps, epsb = k["ones"], k["onesb"], k["eps"], k["epsb"]
            gout, goutb = P.sb([128, NH], F32, "gout")
            ph.dma(gout[:, 0:cfg.HM], io["moba_out_norm_g"].rearrange("(c p) -> p c", p=128), writes=[goutb], allow_slow_non_contiguous=True)
            ph.dma(gout[:, cfg.HM:NH], io["sb_out_norm_g"].rearrange("(c p) -> p c", p=128), writes=[goutb], allow_slow_non_contiguous=True)
            tsl = slice(g * GT, (g + 1) * GT)
            OTs, OTsb = P.sb([128, NH, GT], BF16, "OTs")
            ph.dma(OTs[:], scr["OT"][:, :, tsl].rearrange("h p t -> p h t"), writes=[OTsb])
            sqt = Ring([P.sb([128, 512], BF16, "sqt") for _ in range(2)])
            ssq = Ring([P.ps([128, 512], F32, "ssq") for _ in range(2)])
            rs = Ring([P.sb([128, 512], F32, "rs") for _ in range(2)])
            for sub in range(GT // 512):
                ss_ = slice(sub * 512, (sub + 1) * 512)
                for (h0, h1) in ((0, cfg.HM), (cfg.HM, NH)):
                    sp_, spb = ssq.next()
                    for h in range(h0, h1):
                        sq, sqb = sqt.next()
                        ph.op("act", lambda e, sq=sq, h=h, ss_=ss_: e.activation(sq[:], OTs[:, h, ss_], AF.Square), reads=[OTsb], writes=[sqb])
                        ph.op("pe", lambda e, sq=sq, sp_=sp_, h=h, h0=h0, h1=h1: e.matmul(sp_[:], lhsT=ones[:], rhs=sq[:], start=(h == h0), stop=(h == h1 - 1)),
                              reads=[sqb, onesb], writes=[spb])
                    r, rb = rs.next()
                    ph.op("act", lambda e, r=r, sp_=sp_: e.activation(r[:], sp_[:], AF.Sqrt, bias=eps[:, 0:1], scale=1.0 / W), reads=[spb, epsb], writes=[rb])
                    ph.op("dve", lambda e, r=r: e.reciprocal(r[:], r[:]), reads=[rb], writes=[rb])
                    for h in range(h0, h1):
                        ph.op("dve", lambda e, r=r, h=h, ss_=ss_: e.scalar_tensor_tensor(OTs[:, h, ss_], OTs[:, h, ss_], gout[:, h:h + 1], r[:], ALU.mult, ALU.mult),
                              reads=[rb, goutb], writes=[OTsb])
            res = {"wst_chunks": 16, "wst": Ring([P.sb([128, 16, 256], F32, "wst") for _ in range(4)])}
            wring = Ring([P.sb([128, DC, 256], BF16, "wbf") for _ in range(3)])
            mm = Ring([P.ps([128, 512], F32, "mm") for _ in range(3)])
            xr = Ring([P.sb([128, 256], F32, "xr") for _ in range(6)])
            cast_rr = Ring(["act", "dve"])
            for j in range(D // 256):
                wbf, wbfb = wring.next()
                load_w_bf16(ph, res, io["w_out"][:, j * 256:(j + 1) * 256], DC, 256, wbf, wbfb, cast_rr)
                for t in range(GT // 128):
                    rows = slice(g * GT + t * 128, g * GT + (t + 1) * 128)
                    x_, xb_ = xr.next()
                    ph.dma(x_[:], io["xq"][rows, j * 256:(j + 1) * 256], writes=[xb_])
                    ps, psb = mm.next()
                    for c in range(DC):
                        ph.op("pe", lambda e, ps=ps, c=c, t=t, wbf=wbf: e.matmul(ps[:, 0:256], lhsT=OTs[:, c, t * 128:(t + 1) * 128], rhs=wbf[:, c, :],
                                                                              start=(c == 0), stop=(c == DC - 1)), reads=[OTsb, wbfb], writes=[psb])
                    ph.op("dve", lambda e, x_=x_, ps=ps: e.tensor_tensor(x_[:], ps[:, 0:256], x_[:], ALU.add), reads=[psb], writes=[xb_])
                    ph.dma(scr["x1"][rows, j * 256:(j + 1) * 256], x_[:], reads=[xb_], q="pool")
            ph.emit()


def phase_E(nc, cfg, io, scr):
    D, DC, TQ, NM = cfg.D, cfg.DC, cfg.TQ, cfg.NMEM
    scale = 128 ** -0.5
    with ExitStack() as st:
        P = Pools(nc, st)
        ph = Phase(nc, "E0")
        k = std_consts(nc, ph, P, io)
        ones, onesb, eps, epsb = k["ones"], k["onesb"], k["eps"], k["epsb"]
        gT, gTb = load_vecT(nc, ph, P, io["norm_mem_g"], DC, "gT")
        gk, gkb = load_vecT(nc, ph, P, io["xattn_k_norm_g"], 1, "gk")
        res = hT_res(P, D)
        res["eps"], res["epsb"] = eps, epsb
        mT, mTb = P.sb([128, DC, NM], BF16, "mT")
        build_hT(nc, cfg, ph, P, st, io["memb"], NM, gT, gTb, k["ident"], k["identb"], mT, mTb, res)
        nr = mk_norm_res(P)
        wring = Ring([P.sb([128, DC, 256], BF16, "wbf") for _ in range(2)])
        mm = Ring([P.ps([128, 512], F32, "mm") for _ in range(2)])
        obf = Ring([P.sb([128, NM], BF16, "obf") for _ in range(2)])
        vbf = Ring([P.sb([128, 256], BF16, "vbf") for _ in range(2)])
        cast_rr = Ring(["act", "dve"])
        for j in range(4):
            wbf, wbfb = wring.next()
            load_w_bf16(ph, res, io["w_xkv"][:, j * 256:(j + 1) * 256], DC, 256, wbf, wbfb, cast_rr)
            if j < 2:
                for ct in range(2):
                    h = j * 2 + ct
                    ps, psb = mm.next()
                    for c in range(DC):
                        ph.op("pe", lambda e, ps=ps, c=c, ct=ct, wbf=wbf: e.matmul(ps[:, 0:NM], lhsT=wbf[:, c, ct * 128:(ct + 1) * 128], rhs=mT[:, c, :],
                                                                                 start=(c == 0), stop=(c == DC - 1)), reads=[mTb, wbfb], writes=[psb])
                    o, ob = obf.next()
                    qknorm_evac(ph, ps, psb, NM, gk, gkb, ones, onesb, eps, epsb, nr, o[:], ob)
                    ph.dma(scr["kTx"][h], o[:], reads=[ob], q="pool")
            else:
                for t in range(NM // 128):
                    ps, psb = mm.next()
                    for c in range(DC):
                        ph.op("pe", lambda e, ps=ps, c=c, t=t, wbf=wbf: e.matmul(ps[:, 0:256], lhsT=mT[:, c, t * 128:(t + 1) * 128], rhs=wbf[:, c, :],
                                                                              start=(c == 0), stop=(c == DC - 1)), reads=[mTb, wbfb], writes=[psb])
                    v_, vb_ = vbf.next()
                    ph.op("act", lambda e, v_=v_, ps=ps: e.copy(v_[:], ps[:, 0:256]), reads=[psb], writes=[vb_])
                    ph.dma(scr["vx"][t * 128:(t + 1) * 128, (j - 2) * 256:(j - 1) * 256], v_[:], reads=[vb_], q="pool")
        ph.emit()
    GT = 512
    MT = NM // 128
    for g in range(TQ // GT):
        with ExitStack() as st:
            P = Pools(nc, st)
            ph = Phase(nc, "E1_%d" % g)
            k = std_consts(nc, ph, P, io)
            ones, onesb, eps, epsb = k["ones"], k["onesb"], k["eps"], k["epsb"]
            gT, gTb = load_vecT(nc, ph, P, io["norm_xattn_g"], DC, "gT")
            gq, gqb = load_vecT(nc, ph, P, io["xattn_q_norm_g"], 1, "gq")
            kTx, kTxb = P.sb([128, 4, NM], BF16, "kTx")
            ph.dma(kTx[:], scr["kTx"].rearrange("h p m -> p h m"), writes=[kTxb])
            vx, vxb = P.sb([128, MT, 512], BF16, "vx")
            ph.dma(vx[:], scr["vx"].rearrange("(t p) c -> p t c", p=128), writes=[vxb])
            res = hT_res(P, D, nxt=1)
            res["eps"], res["epsb"] = eps, epsb
            hT, hTb = P.sb([128, DC, GT], BF16, "hT")
            build_hT(nc, cfg, ph, P, st, scr["x1"][g * GT:(g + 1) * GT, :], GT, gT, gTb, k["ident"], k["identb"], hT, hTb, res)
            nr = mk_norm_res(P)
            wring = Ring([P.sb([128, DC, 256], BF16, "wbf") for _ in range(2)])
            mm = Ring([P.ps([128, 512], F32, "mm") for _ in range(2)])
            qTx, qTxb = P.sb([128, 4, GT], BF16, "qTx")
            cast_rr = Ring(["act", "dve"])
            for j in range(2):
                wbf, wbfb = wring.next()
                load_w_bf16(ph, res, io["w_xq"][:, j * 256:(j + 1) * 256], DC, 256, wbf, wbfb, cast_rr)
                for ct in range(2):
                    h = j * 2 + ct
                    ps, psb = mm.next()
                    for c in range(DC):
                        ph.op("pe", lambda e, ps=ps, c=c, ct=ct, wbf=wbf: e.matmul(ps[:, 0:GT], lhsT=wbf[:, c, ct * 128:(ct + 1) * 128], rhs=hT[:, c, :],
                                                                                 start=(c == 0), stop=(c == DC - 1)), reads=[hTb, wbfb], writes=[psb])
                    qknorm_evac(ph, ps, psb, GT, gq, gqb, ones, onesb, eps, epsb, nr, qTx[:, h, :], qTxb)
            OTx, OTxb = P.sb([128, 4, GT], BF16, "OTx")
            pbf = Ring([P.sb([128, GT], BF16, "pbf") for _ in range(2)])
            rd, rdb = P.sb([128, GT], F32, "rd")
            ops_, opsb = P.ps([128, 512], F32, "ops")
            dps, dpsb = P.ps([128, 512], F32, "dps")
            for h in range(4):
                for m in range(MT):
                    S, Sb = mm.next()
                    ph.op("pe", lambda e, S=S, h=h, m=m: e.matmul(S[:, 0:GT], lhsT=kTx[:, h, m * 128:(m + 1) * 128], rhs=qTx[:, h, :], start=True, stop=True),
                          reads=[kTxb, qTxb], writes=[Sb])
                    Pt, Ptb = pbf.next()
                    ph.op("act", lambda e, Pt=Pt, S=S: e.activation(Pt[:], S[:, 0:GT], AF.Exp, scale=scale), reads=[Sb], writes=[Ptb])
                    ph.op("pe", lambda e, Pt=Pt, h=h, m=m: e.matmul(ops_[:, 0:GT], lhsT=vx[:, m, h * 128:(h + 1) * 128], rhs=Pt[:], start=(m == 0), stop=(m == MT - 1)),
                          reads=[vxb, Ptb], writes=[opsb])
                    ph.op("pe", lambda e, Pt=Pt, m=m: e.matmul(dps[:, 0:GT], lhsT=ones[:], rhs=Pt[:], start=(m == 0), stop=(m == MT - 1)),
                          reads=[onesb, Ptb], writes=[dpsb])
                ph.op("dve", lambda e: e.reciprocal(rd[:], dps[:, 0:GT]), reads=[dpsb], writes=[rdb])
                ph.op("dve", lambda e, h=h: e.tensor_tensor(OTx[:, h, :], ops_[:, 0:GT], rd[:], ALU.mult), reads=[opsb, rdb], writes=[OTxb])
            wxo = Ring([P.sb([128, 4, 256], BF16, "wxo") for _ in range(3)])
            wxs = Ring([P.sb([128, 4, 256], F32, "wxs") for _ in range(3)])
            xr = Ring([P.sb([128, 256], F32, "xr") for _ in range(6)])
            for j in range(D // 256):
                ws, wsb = wxs.next()
                ph.dma(ws[:], io["w_xo"][:, j * 256:(j + 1) * 256].rearrange("(c p) n -> p c n", p=128), writes=[wsb])
                wb, wbb = wxo.next()
                ph.op("act", lambda e, wb=wb, ws=ws: e.copy(wb[:], ws[:]), reads=[wsb], writes=[wbb])
                for t in range(GT // 128):
                    rows = slice(g * GT + t * 128, g * GT + (t + 1) * 128)
                    x_, xb_ = xr.next()
                    ph.dma(x_[:], scr["x1"][rows, j * 256:(j + 1) * 256], writes=[xb_])
                    ps, psb = mm.next()
                    for h in range(4):
                        ph.op("pe", lambda e, ps=ps, h=h, t=t, wb=wb: e.matmul(ps[:, 0:256], lhsT=OTx[:, h, t * 128:(t + 1) * 128], rhs=wb[:, h, :],
                                                                            start=(h == 0), stop=(h == 3)), reads=[OTxb, wbb], writes=[psb])
                    ph.op("dve", lambda e, x_=x_, ps=ps: e.tensor_tensor(x_[:], ps[:, 0:256], x_[:], ALU.add), reads=[psb], writes=[xb_])
                    ph.dma(scr["x2"][rows, j * 256:(j + 1) * 256], x_[:], reads=[xb_], q="pool")
            ph.emit()


def phase_F0(nc, cfg, io, scr):
    D, DC = cfg.D, cfg.DC
    NCH = cfg.NE // 128
    CPP = 16
    for g in range(NCH // CPP):
        with ExitStack() as st:
            P = Pools(nc, st)
            ph = Phase(nc, "F0_%d" % g)
            ident, identb = load_const(ph, P, io["ident_bf"], [128, 128], BF16, "ident")
            ut = Ring([P.sb([128, D], F32, "ut") for _ in range(2)])
            ub = Ring([P.sb([128, D], BF16, "ub") for _ in range(2)])
            vt = Ring([P.sb([128, D], F32, "vt") for _ in range(2)])
            vb = Ring([P.sb([128, D], BF16, "vb") for _ in range(2)])
            uTs = Ring([P.sb([128, DC, 128], BF16, "uTs") for _ in range(2)])
            tp = Ring([P.ps([128, 1024], BF16, "tp") for _ in range(3)])
            ev = Ring(["dve", "act"])
            for i in range(g * CPP, (g + 1) * CPP):
                rows = slice(i * 128, (i + 1) * 128)
                u_, u_b = ut.next()
                ph.dma(u_[:], io["peer_u"][rows, :], writes=[u_b])
                ubf, ubfb = ub.next()
                ph.op("act", lambda e, ubf=ubf, u_=u_: e.copy(ubf[:], u_[:]), reads=[u_b], writes=[ubfb])
                uo, uob = uTs.next()
                for c0 in range(0, DC, 8):
                    nch = min(8, DC - c0)
                    t_, t_b = tp.next()
                    for k in range(nch):
                        c = c0 + k
                        ph.op("pe", lambda e, t_=t_, ubf=ubf, c=c, k=k: e.transpose(t_[:, k * 128:(k + 1) * 128], ubf[:, c * 128:(c + 1) * 128], ident[:]),
                              reads=[ubfb, identb], writes=[t_b])
                    eng = ev.next()
                    if eng == "act":
                        ph.op("act", lambda e, uo=uo, t_=t_, c0=c0, nch=nch: e.copy(uo[:, c0:c0 + nch, :], t_[:, 0:nch * 128].rearrange("p (c n) -> p c n", n=128)),
                              reads=[t_b], writes=[uob])
                    else:
                        ph.op("dve", lambda e, uo=uo, t_=t_, c0=c0, nch=nch: e.tensor_copy(uo[:, c0:c0 + nch, :], t_[:, 0:nch * 128].rearrange("p (c n) -> p c n", n=128)),
                              reads=[t_b], writes=[uob])
                ph.dma(scr["uT"][i], uo[:], reads=[uob], q="pool")
                v_, v_b = vt.next()
                ph.dma(v_[:], io["peer_v"][rows, :], writes=[v_b])
                vbf, vbfb = vb.next()
                ph.op("dve", lambda e, vbf=vbf, v_=v_: e.tensor_copy(vbf[:], v_[:]), reads=[v_b], writes=[vbfb])
                ph.dma(scr["vbf"][rows, :], vbf[:], reads=[vbfb], q="pool")
            ph.emit()


def phase_F1a(nc, cfg, io, scr):
    D, DC, TQ = cfg.D, cfg.DC, cfg.TQ
    GT = min(512, TQ)
    NTL = GT // 128
    NSL = 4
    for g in range(TQ // GT):
        with ExitStack() as st:
            P = Pools(nc, st)
            ph = Phase(nc, "F1a_%d" % g)
            k = std_consts(nc, ph, P, io)
            eps, epsb = k["eps"], k["epsb"]
            identf, identfb = load_const(ph, P, io["ident_f32"], [128, 128], F32, "identf")
            gT, gTb = load_vecT(nc, ph, P, io["norm_ffn_g"], DC, "gT")
            res = hT_res(P, D, nxt=1, nxs=1)
            res["eps"], res["epsb"] = eps, epsb
            hT, hTb = P.sb([128, DC, GT], BF16, "hT")
            build_hT(nc, cfg, ph, P, st, scr["x2"][g * GT:(g + 1) * GT, :], GT, gT, gTb, k["ident"], k["identb"], hT, hTb, res)
            for hf in range(GT // 256):
                ph.dma(scr["hTf"][g * (GT // 256) + hf], hT[:, :, hf * 256:(hf + 1) * 256], reads=[hTb], q="pool")
            skf, skfb = P.sb([128, 16, 128], F32, "skf")
            ph.dma(skf[:], io["peer_sub_keys"].rearrange("a k c -> k a c"), writes=[skfb])
            skT, skTb = P.sb([128, 16, 128], BF16, "skT")
            mm = Ring([P.ps([128, 512], F32, "mm") for _ in range(2)])
            sps = Ring([P.ps([128, 512], F32, "sps") for _ in range(2)])
            for a0 in range(0, 16, 4):
                ps, psb = mm.next()
                for a in range(4):
                    ph.op("pe", lambda e, ps=ps, a=a, a0=a0: e.transpose(ps[:, a * 128:(a + 1) * 128], skf[:, a0 + a, :], identf[:]),
                          reads=[skfb, identfb], writes=[psb])
                ph.op("dve", lambda e, ps=ps, a0=a0: e.tensor_copy(skT[:, a0:a0 + 4, :], ps[:].rearrange("p (a k) -> p a k", k=128)),
                      reads=[psb], writes=[skTb])
            wring = Ring([P.sb([128, DC, 256], BF16, "wbf") for _ in range(2)])
            cast_rr = Ring(["act", "dve"])
            qTr = Ring([P.sb([128, GT], BF16, "qT") for _ in range(2)])
            sall = [P.sb([128, 16, 128], F32, "sall") for _ in range(NTL)]
            for j in range(8):
                wbf, wbfb = wring.next()
                load_w_bf16(ph, res, io["w_peer_q"][:, j * 256:(j + 1) * 256], DC, 256, wbf, wbfb, cast_rr)
                for ct in range(2):
                    hp = 2 * j + ct
                    ps, psb = mm.next()
                    for c in range(DC):
                        ph.op("pe", lambda e, ps=ps, c=c, ct=ct, wbf=wbf: e.matmul(ps[:, 0:GT], lhsT=wbf[:, c, ct * 128:(ct + 1) * 128], rhs=hT[:, c, :],
                                                                                 start=(c == 0), stop=(c == DC - 1)), reads=[hTb, wbfb], writes=[psb])
                    q_, q_b = qTr.next()
                    ph.op("act", lambda e, q_=q_, ps=ps: e.copy(q_[:], ps[:, 0:GT]), reads=[psb], writes=[q_b])
                    for t in range(NTL):
                        s_, s_b = sps.next()
                        ph.op("pe", lambda e, s_=s_, q_=q_, t=t, hp=hp: e.matmul(s_[:, 0:128], lhsT=q_[:, t * 128:(t + 1) * 128], rhs=skT[:, hp, :], start=True, stop=True),
                              reads=[q_b, skTb], writes=[s_b])
                        ph.op("dve", lambda e, s_=s_, t=t, hp=hp: e.tensor_copy(sall[t][0][:, hp, :], s_[:, 0:128]), reads=[s_b], writes=[sall[t][1]])
            slots = []
            for _ in range(NSL):
                slots.append({
                    "v01": P.sb([128, 2, 16], F32, "v01"), "wk": [P.sb([128, 128], F32, "wk") for _ in range(2)],
                    "cand": P.sb([128, 16, 16], F32, "cand"), "c24": P.sb([128, 24], F32, "c24"),
                    "w2": P.sb([128, 256], F32, "w2"), "w3": P.sb([128, 256], F32, "w3"),
                    "sc": P.sb([128, 8], F32, "sc"), "e16": P.sb([128, 16], F32, "e16"),
                })
            s1pp = [P.sb([128, 8, 128], F32, "s1pp") for _ in range(NTL)]
            thrr = [P.sb([128, 8], F32, "thr") for _ in range(NTL)]
            kapr = [P.sb([128, 8], F32, "kap") for _ in range(NTL)]

            def chain(t, h, S):
                sa, sab = sall[t]
                v01, v01b = S["v01"]
                cand, candb = S["cand"]
                c24, c24b = S["c24"]
                w2, w2b = S["w2"]
                w3, w3b = S["w3"]
                sc, scb = S["sc"]
                e16, e16b = S["e16"]
                thr, thrb = thrr[t]
                steps = []
                for pp in range(2):
                    wk, wkb = S["wk"][pp]
                    sx = sa[:, 2 * h + pp, :]
                    steps.append(lambda sx=sx, pp=pp: ph.op("dve", lambda e: e.max(v01[:, pp, 0:8], sx), reads=[sab], writes=[v01b]))
                    steps.append(lambda sx=sx, pp=pp, wk=wk, wkb=wkb: ph.op("dve", lambda e: e.match_replace(wk[:], v01[:, pp, 0:8], sx, -1.0e30), reads=[sab, v01b], writes=[wkb]))
                    steps.append(lambda pp=pp, wk=wk, wkb=wkb: ph.op("dve", lambda e: e.max(v01[:, pp, 8:16], wk[:]), reads=[wkb], writes=[v01b]))
                cf = cand[:].rearrange("p a b -> p (a b)")
                steps.append(lambda: ph.op("dve", lambda e: e.tensor_tensor(cand[:], v01[:, 0, :].unsqueeze(2).to_broadcast([128, 16, 16]),
                                                                           v01[:, 1, :].unsqueeze(1).to_broadcast([128, 16, 16]), ALU.add), reads=[v01b], writes=[candb]))
                steps.append(lambda: ph.op("dve", lambda e: e.max(c24[:, 0:8], cf), reads=[candb], writes=[c24b]))
                steps.append(lambda: ph.op("dve", lambda e: e.match_replace(w2[:], c24[:, 0:8], cf, -1.0e30), reads=[candb, c24b], writes=[w2b]))
                steps.append(lambda: ph.op("dve", lambda e: e.max(c24[:, 8:16], w2[:]), reads=[w2b], writes=[c24b]))
                steps.append(lambda: ph.op("dve", lambda e: e.match_replace(w3[:], c24[:, 8:16], w2[:], -1.0e30), reads=[w2b, c24b], writes=[w3b]))
                steps.append(lambda: ph.op("dve", lambda e: e.max(c24[:, 16:24], w3[:]), reads=[w3b], writes=[c24b]))
                steps.append(lambda: ph.op("dve", lambda e: e.tensor_scalar(sc[:, 0:1], c24[:, 0:1], -1.0, None, ALU.mult), reads=[c24b], writes=[scb]))
                steps.append(lambda: ph.op("act", lambda e: e.activation(e16[:], c24[:, 0:16], AF.Exp, bias=sc[:, 0:1], scale=1.0, accum_out=sc[:, 1:2]),
                                           reads=[c24b, scb], writes=[e16b, scb]))
                steps.append(lambda: ph.op("act", lambda e: e.activation(sc[:, 2:3], sc[:, 1:2], AF.Ln), reads=[scb], writes=[scb]))
                steps.append(lambda: ph.op("dve", lambda e: e.tensor_tensor(sc[:, 3:4], sc[:, 0:1], sc[:, 2:3], ALU.subtract), reads=[scb], writes=[scb]))
                steps.append(lambda: ph.op("dve", lambda e: e.tensor_tensor(sc[:, 4:5], c24[:, 15:16], c24[:, 16:17], ALU.add), reads=[scb, c24b], writes=[scb]))
                steps.append(lambda: ph.op("dve", lambda e: e.tensor_scalar(sc[:, 5:6], sc[:, 4:5], 0.5, None, ALU.mult), reads=[scb], writes=[scb]))
                steps.append(lambda: ph.op("dve", lambda e: e.tensor_tensor(thr[:, h:h + 1], sc[:, 5:6], sc[:, 3:4], ALU.add), reads=[scb], writes=[thrb]))
                steps.append(lambda: ph.op("dve", lambda e: e.tensor_scalar(s1pp[t][0][:, h, :], sa[:, 2 * h + 1, :], sc[:, 5:6], None, ALU.subtract),
                                           reads=[scb, sab], writes=[s1pp[t][1]]))
                return steps

            jobs = [(t, h) for t in range(NTL) for h in range(8)]
            for j0 in range(0, len(jobs), NSL):
                chains = [chain(t, h, slots[si]) for si, (t, h) in enumerate(jobs[j0:j0 + NSL])]
                for step in range(max(len(c) for c in chains)):
                    for c in chains:
                        if step < len(c):
                            c[step]()
            for t in range(NTL):
                ti = g * NTL + t
                ph.op("act", lambda e, t=t: e.activation(kapr[t][0][:], thrr[t][0][:], AF.Exp), reads=[thrr[t][1]], writes=[kapr[t][1]])
                ph.dma(scr["sall"][ti], sall[t][0][:].rearrange("p (h two) k -> p h two k", two=2)[:, :, 0, :], reads=[sall[t][1]], q="pool")
                ph.dma(scr["s1p"][ti], s1pp[t][0][:], reads=[s1pp[t][1]], q="pool")
                ph.dma(scr["thr"][ti], kapr[t][0][:], reads=[kapr[t][1]], q="pool")
            ph.emit()


def phase_F1b(nc, cfg, io, scr):
    D, DC, TQ = cfg.D, cfg.DC, cfg.TQ
    GT = 256
    NCH = cfg.NE // 128
    bw = min(512, D // 2)
    bpr = 2 if D >= 2048 else 1
    npair = 2
    nbk = bpr * npair
    rw = bpr * bw
    rounds_total = 2 * (D // rw)
    rounds_per_iter = (rounds_total + 3) // 4
    for g in range(TQ // GT):
        with ExitStack() as st:
            P = Pools(nc, st)
            ph = Phase(nc, "F1b_%d" % g)
            identb_, identbb = load_const(ph, P, io["ident_bf"], [128, 128], BF16, "identb")
            hT, hTb = load_const(ph, P, scr["hTf"][g], [128, DC, GT], BF16, "hT")
            sall, s1p, thr, osb = [], [], [], []
            for t in range(2):
                ti = g * 2 + t
                sall.append(load_const(ph, P, scr["sall"][ti], [128, 8, 128], F32, "sall"))
                s1p.append(load_const(ph, P, scr["s1p"][ti], [128, 8, 128], F32, "s1p"))
                thr.append(load_const(ph, P, scr["thr"][ti], [128, 8], F32, "thr"))
                osb.append(load_const(ph, P, scr["x2"][ti * 128:(ti + 1) * 128, :], [128, D], F32, "osb"))
            kdiag, kdiagb = P.sb([128, 2, 8, 128], BF16, "kdiag")
            for t in range(2):
                for h in range(8):
                    ph.op("dve", lambda e, t=t, h=h: e.tensor_scalar(kdiag[:, t, h, :], identb_[:], thr[t][0][:, h:h + 1], None, ALU.mult),
                          reads=[identbb, thr[t][1]], writes=[kdiagb])
            uTr = Ring([P.sb([128, DC, 128], BF16, "uT") for _ in range(2)])
            vbr = Ring([P.sb([128, D], BF16, "vb") for _ in range(9)])
            aps = Ring([P.ps([128, 512], F32, "aps") for _ in range(2)])
            gpr = Ring([P.ps([128, 512], F32, "gps") for _ in range(2)])
            ops_ = [P.ps([128, 512], F32, "ops") for _ in range(nbk)]
            actr = Ring([P.sb([128, GT], BF16, "actT") for _ in range(3)])
            Dr = Ring([P.sb([128, 8, 128], F32, "Dt") for _ in range(2)])
            Er = Ring([P.sb([128, 8, 128], F32, "Et") for _ in range(2)])
            Fr = Ring([P.sb([128, 8, 128], BF16, "Ft") for _ in range(10)])
            GAr = Ring([P.sb([128, 4, GT], BF16, "GA") for _ in range(2)])
            st_ = {}

            def gate(i):
                Gs = []
                for t in range(2):
                    sa, sab = sall[t]
                    Dt, Dtb = Dr.next()
                    s0i = sa[:, :, i:i + 1].to_broadcast([128, 8, 128])
                    ph.op("pool", lambda e, Dt=Dt, s0i=s0i, t=t: e.tensor_tensor(Dt[:], s1p[t][0][:], s0i, ALU.add), reads=[s1p[t][1], sab], writes=[Dtb])
                    Et, Etb = Er.next()
                    ph.op("act", lambda e, Et=Et, Dt=Dt: e.activation(Et[:], Dt[:], AF.Exp), reads=[Dtb], writes=[Etb])
                    Ft, Ftb = Fr.next()
                    ph.op("dve", lambda e, Ft=Ft, Dt=Dt, Et=Et: e.scalar_tensor_tensor(Ft[:].rearrange("p h j -> p (h j)"), Dt[:].rearrange("p h j -> p (h j)"), 0.0,
                                                                                     Et[:].rearrange("p h j -> p (h j)"), ALU.is_ge, ALU.mult),
                          reads=[Dtb, Etb], writes=[Ftb])
                    Gs.append((Ft, Ftb))
                st_[("G", i)] = Gs

            def s1(i):
                uT, uTb = uTr.next()
                ph.dma(uT[:], scr["uT"][i], writes=[uTb])
                vb, vbb = vbr.next()
                ph.dma(vb[:], scr["vbf"][i * 128:(i + 1) * 128, :], writes=[vbb])
                st_[("vb", i)] = (vb, vbb)
                A, Ab = aps.next()
                for c in range(DC):
                    ph.op("pe", lambda e, c=c: e.matmul(A[:, 0:GT], lhsT=uT[:, c, :], rhs=hT[:, c, :], start=(c == 0), stop=(c == DC - 1)),
                          reads=[uTb, hTb], writes=[Ab])
                aT, aTb = actr.next()
                ph.op("act", lambda e: e.activation(aT[:], A[:, 0:GT], AF.Gelu), reads=[Ab], writes=[aTb])
                st_[("a", i)] = (aT, aTb)

            def s2(i):
                c4 = i % 4
                if c4 == 0:
                    st_["GA"] = GAr.next()
                GA, GAb = st_["GA"]
                aT, aTb = st_.pop(("a", i))
                Gs = st_.pop(("G", i))
                for t in range(2):
                    G, Gb = Gs[t]
                    gps, gpsb = gpr.next()
                    for h in range(8):
                        ph.op("pe", lambda e, G=G, gps=gps, h=h, t=t: e.matmul(gps[:, 0:128], lhsT=G[:, h, :], rhs=kdiag[:, t, h, :], start=(h == 0), stop=(h == 7)),
                              reads=[Gb, kdiagb], writes=[gpsb])
                    ph.op("dve", lambda e, t=t, gps=gps: e.tensor_tensor(GA[:, c4, t * 128:(t + 1) * 128], gps[:, 0:128], aT[:, t * 128:(t + 1) * 128], ALU.mult),
                          reads=[gpsb, aTb], writes=[GAb])

            pending = []
            add_q = []

            def push_s3(cg):
                GA, GAb = st_["GA"]
                vbs = [st_.pop(("vb", cg * 4 + c4)) for c4 in range(4)]
                for t in range(2):
                    for col0 in range(0, D, rw):
                        pending.append((GA, GAb, vbs, t, col0))

            rr_pair = [0]

            def emit_round():
                GA, GAb, vbs, t, col0 = pending.pop(0)
                pair = rr_pair[0] % npair
                rr_pair[0] += 1
                banks = ops_[pair * bpr:(pair + 1) * bpr]
                for c4 in range(4):
                    vb, vbb = vbs[c4]
                    for b in range(bpr):
                        cc = col0 + b * bw
                        ph.op("pe", lambda e, b=b, c4=c4, vb=vb, cc=cc: e.matmul(banks[b][0][:, 0:bw], lhsT=GA[:, c4, t * 128:(t + 1) * 128], rhs=vb[:, cc:cc + bw],
                                                                            start=(c4 == 0), stop=(c4 == 3)), reads=[GAb, vbb], writes=[banks[b][1]])
                flush_adds()
                for b in range(bpr):
                    cc = col0 + b * bw
                    add_q.append((banks[b], t, cc))

            def flush_adds():
                while add_q:
                    (bk, bkb), t, cc = add_q.pop(0)
                    ph.op("dve", lambda e, bk=bk, t=t, cc=cc: e.tensor_tensor(osb[t][0][:, cc:cc + bw], bk[:, 0:bw], osb[t][0][:, cc:cc + bw], ALU.add),
                          reads=[bkb], writes=[osb[t][1]])

            for k in range(NCH + 4):
                if k < NCH:
                    gate(k)
                if 0 <= k - 1 < NCH:
                    s1(k - 1)
                if 0 <= k - 3 < NCH:
                    s2(k - 3)
                    if (k - 3) % 4 == 3:
                        push_s3((k - 3) // 4)
                for _ in range(rounds_per_iter):
                    if pending:
                        emit_round()
            while pending:
                emit_round()
            flush_adds()
            for t in range(2):
                ti = g * 2 + t
                ph.dma(io["y"][ti * 128:(ti + 1) * 128, :], osb[t][0][:], reads=[osb[t][1]], q="pool")
            ph.emit()


def build_program(cfg, debug=()):
    nc = bass.Bass("TRN2", target_bir_lowering=False)
    io, scr = declare_io(nc, cfg, debug=debug)
    clear_all_sems(nc)
    phase_A(nc, cfg, io, scr)
    f0 = F0Sched(cfg)
    phase_B(nc, cfg, io, scr, f0)
    assert f0.next_chunk == cfg.NE // 128
    phase_C(nc, cfg, io, scr, None)
    phase_D(nc, cfg, io, scr)
    phase_E(nc, cfg, io, scr)
    phase_F1a(nc, cfg, io, scr)
    phase_F1b(nc, cfg, io, scr)
    return nc, io


def kernel(x, mem, norm_mix_g, w_in, moba_q_norm_g, moba_k_norm_g, moba_out_norm_g, sb_out_norm_g, w_out,
           norm_xattn_g, norm_mem_g, w_xq, w_xkv, xattn_q_norm_g, xattn_k_norm_g, w_xo, norm_ffn_g,
           w_peer_q, peer_sub_keys, peer_u, peer_v):
    x = np.asarray(x, np.float32)
    B, T, D = x.shape
    cfg = Cfg(D=D, T=T, B=B, NMEM=np.asarray(mem).shape[1])
    nc, io = build_program(cfg)

    def f(a):
        return np.ascontiguousarray(np.asarray(a, np.float32)[0])

    shared = {
        "norm_mix_g": f(norm_mix_g), "w_in": f(w_in), "moba_q_norm_g": f(moba_q_norm_g), "moba_k_norm_g": f(moba_k_norm_g),
        "moba_out_norm_g": f(moba_out_norm_g), "sb_out_norm_g": f(sb_out_norm_g), "w_out": f(w_out),
        "norm_xattn_g": f(norm_xattn_g), "norm_mem_g": f(norm_mem_g), "w_xq": f(w_xq), "w_xkv": f(w_xkv),
        "xattn_q_norm_g": f(xattn_q_norm_g), "xattn_k_norm_g": f(xattn_k_norm_g), "w_xo": f(w_xo),
        "norm_ffn_g": f(norm_ffn_g), "w_peer_q": f(w_peer_q),
        "peer_sub_keys": f(peer_sub_keys).reshape(16, 128, 128), "peer_u": f(peer_u), "peer_v": f(peer_v),
    }
    consts = [host_consts(cfg, p) for p in range(2)]
    mem = np.asarray(mem, np.float32)
    in_maps = []
    rows_of = []
    for core in range(cfg.ncores):
        b, p = core // 2, core % 2
        rows = np.concatenate([np.arange(blk * 256, (blk + 1) * 256) for blk in own_blocks(cfg, p)])
        rows_of.append((b, rows))
        m = dict(shared)
        m["xb"] = np.ascontiguousarray(x[b])
        m["xq"] = np.ascontiguousarray(x[b][rows])
        m["memb"] = np.ascontiguousarray(mem[b])
        for k, v in consts[p].items():
            if k in io["_shapes"]:
                m[k] = v
        in_maps.append(m)
    res = run_bass_kernel_spmd(nc, in_maps, core_ids=list(range(cfg.ncores)))
    out = np.empty((B, T, D), np.float32)
    for core in range(cfg.ncores):
        b, rows = rows_of[core]
        out[b, rows] = np.asarray(res.results[core]["y"], np.float32)
    return out
```
